# Optimizing a Trainium2 kernel written in Bass

```python
import math
import jax, jax.numpy as jnp
from jax import lax
import numpy as np

D_MODEL = 2048
BATCH = 4
SEQ = 4096
DEPTH = 4

PLE_DIM = 256
HEAD_DIM = 128
NSA_HEADS = 8
NSA_KV_GROUPS = 2
NSA_CMP_BLOCK = 32
NSA_CMP_STRIDE = 16
NSA_CMP_HIDDEN = 256
NSA_SLC_BLOCK = 64
NSA_SLC_TOPN = 16
NSA_WINDOW = 512
SB_HEADS = 4
DSA_HEADS = 4
DSA_KV_RANK = 256
DSA_IDX_HEADS = 8
DSA_IDX_DIM = 64
DSA_TOPK_MAX = 256
D_FF = 5632
CONV_WIDTH = 3
REL_BUCKETS = 32
REL_MAX_DIST = 128
Q_BLOCK = 128
SLC_Q_BLOCK = 64

IN_SIZES = (
    3 * D_MODEL,
    NSA_HEADS * HEAD_DIM,
    NSA_KV_GROUPS * HEAD_DIM,
    NSA_KV_GROUPS * HEAD_DIM,
    NSA_KV_GROUPS * HEAD_DIM,
    NSA_KV_GROUPS * HEAD_DIM,
    NSA_KV_GROUPS * HEAD_DIM,
    NSA_KV_GROUPS * HEAD_DIM,
    3 * NSA_HEADS,
    SB_HEADS * HEAD_DIM,
    SB_HEADS * HEAD_DIM,
    SB_HEADS * HEAD_DIM,
    DSA_HEADS * HEAD_DIM,
    DSA_KV_RANK,
    DSA_IDX_HEADS * DSA_IDX_DIM,
    DSA_IDX_DIM,
    DSA_IDX_HEADS,
)
IN_COLS = 3 * D_MODEL + NSA_HEADS * HEAD_DIM + 6 * NSA_KV_GROUPS * HEAD_DIM + 3 * NSA_HEADS + 3 * SB_HEADS * HEAD_DIM + DSA_HEADS * HEAD_DIM + DSA_KV_RANK + DSA_IDX_HEADS * DSA_IDX_DIM + DSA_IDX_DIM + DSA_IDX_HEADS

kernel_name = "hybrid_nsa_stickbreak_dsa_convffn"


def _split_cols(u, sizes):
    outs, off = [], 0
    for s in sizes:
        outs.append(u[..., off:off + s])
        off += s
    return outs


def rmsnorm(x, g, eps=1e-6):
    xf = x.astype(jnp.float32)
    y = xf * lax.rsqrt(jnp.mean(xf * xf, axis=-1, keepdims=True) + eps)
    return (y * g.astype(jnp.float32)).astype(x.dtype)


def masked_softmax(logits, mask):
    l = jnp.where(mask, logits.astype(jnp.float32), -jnp.inf)
    m = jnp.max(l, axis=-1, keepdims=True)
    m = jnp.where(jnp.isfinite(m), m, 0.0)
    e = jnp.where(mask, jnp.exp(l - m), 0.0)
    return e / jnp.maximum(jnp.sum(e, axis=-1, keepdims=True), 1e-30)


def t5_bucket(dist):
    n = jnp.maximum(dist, 0)
    max_exact = REL_BUCKETS // 2
    nf = jnp.maximum(n, 1).astype(jnp.float32)
    large = max_exact + (jnp.log(nf / max_exact) / math.log(REL_MAX_DIST / max_exact)
                         * (REL_BUCKETS - max_exact)).astype(jnp.int32)
    large = jnp.minimum(large, REL_BUCKETS - 1)
    return jnp.where(n < max_exact, n, large)


def _unblock(o, axis):
    o = jnp.moveaxis(o, 0, axis)
    sh = o.shape
    return o.reshape(sh[:axis] + (sh[axis] * sh[axis + 1],) + sh[axis + 2:])


def nsa_mixer(q, k_cmp, v_cmp, k_slc, v_slc, k_win, v_win, g_branch,
              ck_w1, ck_w2, ck_pe, cv_w1, cv_w2, cv_pe, tab):
    B, S, _ = q.shape
    G, R, d = NSA_KV_GROUPS, NSA_HEADS // NSA_KV_GROUPS, HEAD_DIM
    scale = d ** -0.5
    q = q.reshape(B, S, G, R, d).transpose(0, 2, 3, 1, 4)

    def heads_kv(a):
        return a.reshape(B, S, G, d).transpose(0, 2, 1, 3)

    k_cmp, v_cmp, k_slc, v_slc, k_win, v_win = map(heads_kv, (k_cmp, v_cmp, k_slc, v_slc, k_win, v_win))
    t = jnp.arange(S)
    tab_gr = tab.reshape(REL_BUCKETS, G, R)

    nc = (S - NSA_CMP_BLOCK) // NSA_CMP_STRIDE + 1
    starts = jnp.arange(nc) * NSA_CMP_STRIDE
    blk_idx = starts[:, None] + jnp.arange(NSA_CMP_BLOCK)[None, :]

    def compress(a, w1, w2, pe):
        blk = a[:, :, blk_idx] + pe
        return jax.nn.gelu(blk.reshape(B, G, nc, NSA_CMP_BLOCK * d) @ w1) @ w2

    kc = compress(k_cmp, ck_w1, ck_w2, ck_pe)
    vc = compress(v_cmp, cv_w1, cv_w2, cv_pe)
    ends = starts + NSA_CMP_BLOCK - 1
    dist_c = t[:, None] - ends[None, :]
    bias_c = jnp.transpose(tab_gr[t5_bucket(dist_c)], (2, 3, 0, 1))
    logits_c = jnp.einsum('bgrsd,bgcd->bgrsc', q, kc).astype(jnp.float32) * scale + bias_c.astype(jnp.float32)
    p_cmp = masked_softmax(logits_c, dist_c >= 0)
    o_cmp = jnp.einsum('bgrsc,bgcd->bgrsd', p_cmp.astype(vc.dtype), vc)

    nsel = S // NSA_SLC_BLOCK
    n_top = min(NSA_SLC_TOPN, nsel)
    jstart = jnp.arange(nsel) * NSA_SLC_BLOCK
    overlap = ((starts[:, None] < jstart[None, :] + NSA_SLC_BLOCK)
               & (starts[:, None] + NSA_CMP_BLOCK > jstart[None, :])).astype(jnp.float32)
    imp = jnp.einsum('bgrsc,cj->bgsj', p_cmp, overlap)
    jj = jnp.arange(nsel)
    cur = t // NSA_SLC_BLOCK
    valid_blk = jstart[None, :] <= t[:, None]
    forced = (jj[None, :] == 0) | (jj[None, :] == cur[:, None]) | (jj[None, :] == cur[:, None] - 1)
    score = jnp.where(valid_blk, jnp.where(forced, 1e9, imp), -jnp.inf)
    _, sel_idx = lax.top_k(score, n_top)

    k_blocks = k_slc.reshape(B, G, nsel, NSA_SLC_BLOCK, d)
    v_blocks = v_slc.reshape(B, G, nsel, NSA_SLC_BLOCK, d)
    gather_bg = jax.vmap(jax.vmap(lambda kb, ix: kb[ix]))
    g_ar = jnp.arange(G)[None, :, None, None, None]

    def slc_block(i):
        s0 = i * SLC_Q_BLOCK
        qb = lax.dynamic_slice_in_dim(q, s0, SLC_Q_BLOCK, axis=3)
        ib = lax.dynamic_slice_in_dim(sel_idx, s0, SLC_Q_BLOCK, axis=2)
        kg = gather_bg(k_blocks, ib)
        vg = gather_bg(v_blocks, ib)
        tq = s0 + jnp.arange(SLC_Q_BLOCK)
        kpos = ib[..., None] * NSA_SLC_BLOCK + jnp.arange(NSA_SLC_BLOCK)
        dist = tq[None, None, :, None, None] - kpos
        bias = jnp.moveaxis(tab_gr[t5_bucket(dist), g_ar], -1, 2)
        logits = jnp.einsum('bgrqd,bgqnkd->bgrqnk', qb, kg).astype(jnp.float32) * scale + bias.astype(jnp.float32)
        m_ = n_top * NSA_SLC_BLOCK
        p = masked_softmax(logits.reshape(B, G, R, SLC_Q_BLOCK, m_),
                           (dist >= 0).reshape(B, G, 1, SLC_Q_BLOCK, m_))
        return jnp.einsum('bgrqm,bgqmd->bgrqd', p.astype(vg.dtype), vg.reshape(B, G, SLC_Q_BLOCK, m_, d))

    o_slc = _unblock(lax.map(slc_block, jnp.arange(S // SLC_Q_BLOCK)), 3)

    span = NSA_WINDOW + Q_BLOCK
    kw = jnp.pad(k_win, ((0, 0), (0, 0), (NSA_WINDOW, 0), (0, 0)))
    vw = jnp.pad(v_win, ((0, 0), (0, 0), (NSA_WINDOW, 0), (0, 0)))

    def win_block(i):
        s0 = i * Q_BLOCK
        qb = lax.dynamic_slice_in_dim(q, s0, Q_BLOCK, axis=3)
        kb = lax.dynamic_slice_in_dim(kw, s0, span, axis=2)
        vb = lax.dynamic_slice_in_dim(vw, s0, span, axis=2)
        kpos = s0 - NSA_WINDOW + jnp.arange(span)
        tq = s0 + jnp.arange(Q_BLOCK)
        dist = tq[:, None] - kpos[None, :]
        mask = (dist >= 0) & (dist < NSA_WINDOW) & (kpos[None, :] >= 0)
        bias = jnp.transpose(tab_gr[t5_bucket(dist)], (2, 3, 0, 1))
        logits = jnp.einsum('bgrqd,bgkd->bgrqk', qb, kb).astype(jnp.float32) * scale + bias.astype(jnp.float32)
        p = masked_softmax(logits, mask)
        return jnp.einsum('bgrqk,bgkd->bgrqd', p.astype(vb.dtype), vb)

    o_win = _unblock(lax.map(win_block, jnp.arange(S // Q_BLOCK)), 3)

    g = jax.nn.sigmoid(g_branch.reshape(B, S, G, R, 3).transpose(0, 2, 3, 1, 4))
    o = g[..., 0:1] * o_cmp + g[..., 1:2] * o_slc + g[..., 2:3] * o_win
    return o.transpose(0, 3, 1, 2, 4).reshape(B, S, NSA_HEADS * d)


def stick_breaking(q, k, v):
    B, S, _ = q.shape
    H, d = SB_HEADS, HEAD_DIM
    scale = d ** -0.5
    q, k, v = (a.reshape(B, S, H, d).transpose(0, 2, 1, 3) for a in (q, k, v))
    ks = jnp.arange(S)

    def blk(i):
        s0 = i * Q_BLOCK
        qb = lax.dynamic_slice_in_dim(q, s0, Q_BLOCK, axis=2)
        z = jnp.einsum('bhqd,bhkd->bhqk', qb, k).astype(jnp.float32) * scale
        tq = s0 + jnp.arange(Q_BLOCK)
        mask = ks[None, :] < tq[:, None]
        log_keep = jnp.where(mask, jax.nn.log_sigmoid(-z), 0.0)
        after = lax.cumsum(log_keep, axis=3, reverse=True) - log_keep
        w = jnp.where(mask, jnp.exp(jax.nn.log_sigmoid(z) + after), 0.0)
        return jnp.einsum('bhqk,bhkd->bhqd', w.astype(v.dtype), v)

    o = _unblock(lax.map(blk, jnp.arange(S // Q_BLOCK)), 2)
    return o.transpose(0, 2, 1, 3).reshape(B, S, H * d)


def dsa_mixer(q, c_kv, q_idx, k_idx, w_idx, kv_norm, w_uk, w_uv, tab):
    B, S, _ = q.shape
    H, d = DSA_HEADS, HEAD_DIM
    scale = d ** -0.5
    n_keep = min(DSA_TOPK_MAX, S // 4)
    q = q.reshape(B, S, H, d)
    ckv = rmsnorm(c_kv, kv_norm)
    k = ckv @ w_uk
    v = ckv @ w_uv
    qi = q_idx.reshape(B, S, DSA_IDX_HEADS, DSA_IDX_DIM)
    wi = w_idx.astype(jnp.float32) * (DSA_IDX_HEADS ** -0.5) * (DSA_IDX_DIM ** -0.5)
    ks = jnp.arange(S)
    gather_b = jax.vmap(lambda a, ix: a[ix])

    def blk(i):
        s0 = i * Q_BLOCK
        qb = lax.dynamic_slice_in_dim(q, s0, Q_BLOCK, axis=1)
        qib = lax.dynamic_slice_in_dim(qi, s0, Q_BLOCK, axis=1)
        wib = lax.dynamic_slice_in_dim(wi, s0, Q_BLOCK, axis=1)
        tq = s0 + jnp.arange(Q_BLOCK)
        dots = jnp.einsum('bqhe,bse->bqhs', qib, k_idx).astype(jnp.float32)
        score = jnp.einsum('bqh,bqhs->bqs', wib, jax.nn.relu(dots))
        score = jnp.where(ks[None, None, :] <= tq[None, :, None], score, -jnp.inf)
        _, idx = lax.top_k(score, n_keep)
        kg = gather_b(k, idx)
        vg = gather_b(v, idx)
        dist = tq[None, :, None] - idx
        bias = jnp.moveaxis(tab[t5_bucket(dist)], -1, 2)
        logits = jnp.einsum('bqhd,bqnd->bqhn', qb, kg).astype(jnp.float32) * scale + bias.astype(jnp.float32)
        p = masked_softmax(logits, (dist >= 0)[:, :, None, :])
        return jnp.einsum('bqhn,bqnd->bqhd', p.astype(vg.dtype), vg)

    o = _unblock(lax.map(blk, jnp.arange(S // Q_BLOCK)), 1)
    return o.reshape(B, S, H * d)


def token_mixer(h, w_in, w_a, w_b, w_c, w_out, ck_w1, ck_w2, ck_pe, cv_w1, cv_w2, cv_pe,
                kv_norm, w_uk, w_uv, rel_tab):
    B, S, _ = h.shape
    u = h @ w_in
    (g_merge, nsa_q, k_cmp, v_cmp, k_slc, v_slc, k_win, v_win, nsa_g,
     sb_q, sb_k, sb_v, dsa_q, dsa_ckv, idx_q, idx_k, idx_w) = _split_cols(u, IN_SIZES)
    o_a = nsa_mixer(nsa_q, k_cmp, v_cmp, k_slc, v_slc, k_win, v_win, nsa_g,
                    ck_w1, ck_w2, ck_pe, cv_w1, cv_w2, cv_pe, rel_tab[:, :NSA_HEADS])
    o_b = stick_breaking(sb_q, sb_k, sb_v)
    o_c = dsa_mixer(dsa_q, dsa_ckv, idx_q, idx_k, idx_w, kv_norm, w_uk, w_uv, rel_tab[:, NSA_HEADS:])
    g = jax.nn.sigmoid(g_merge.reshape(B, S, 3, D_MODEL))
    y = g[:, :, 0] * (o_a @ w_a) + g[:, :, 1] * (o_b @ w_b) + g[:, :, 2] * (o_c @ w_c)
    return y @ w_out


def conv_ffn(h, w_gate, w_up, w_down, conv_w, conv_b):
    a = h @ w_gate
    a = lax.conv_general_dilated(a, conv_w[:, None, :], window_strides=(1,),
                                 padding=((CONV_WIDTH - 1, 0),),
                                 dimension_numbers=('NWC', 'WIO', 'NWC'),
                                 feature_group_count=D_FF) + conv_b
    return (jax.nn.gelu(a) * (h @ w_up)) @ w_down


def setup_inputs(seed: int = 0) -> dict:
    key = jax.random.key(seed)
    ks = iter(jax.random.split(key, 32))

    def nrm(shape, scale):
        return jax.random.normal(next(ks), shape, jnp.float32) * scale

    L = DEPTH
    return {
        'x': nrm((BATCH, SEQ, D_MODEL), 1.0),
        'p': nrm((DEPTH, BATCH, SEQ, PLE_DIM), 1.0),
        'w_in': nrm((L, D_MODEL, IN_COLS), D_MODEL ** -0.5),
        'norm_mix': 1.0 + nrm((L, D_MODEL), 0.01),
        'norm_ffn': 1.0 + nrm((L, D_MODEL), 0.01),
        'norm_ple': 1.0 + nrm((L, D_MODEL), 0.01),
        'norm_final': 1.0 + nrm((D_MODEL,), 0.01),
        'w_proj_a': nrm((L, NSA_HEADS * HEAD_DIM, D_MODEL), (NSA_HEADS * HEAD_DIM) ** -0.5),
        'w_proj_b': nrm((L, SB_HEADS * HEAD_DIM, D_MODEL), (SB_HEADS * HEAD_DIM) ** -0.5),
        'w_proj_c': nrm((L, DSA_HEADS * HEAD_DIM, D_MODEL), (DSA_HEADS * HEAD_DIM) ** -0.5),
        'w_out': nrm((L, D_MODEL, D_MODEL), D_MODEL ** -0.5),
        'cmp_k_w1': nrm((L, NSA_CMP_BLOCK * HEAD_DIM, NSA_CMP_HIDDEN), (NSA_CMP_BLOCK * HEAD_DIM) ** -0.5),
        'cmp_k_w2': nrm((L, NSA_CMP_HIDDEN, HEAD_DIM), NSA_CMP_HIDDEN ** -0.5),
        'cmp_k_pe': nrm((L, NSA_CMP_BLOCK, HEAD_DIM), 0.1),
        'cmp_v_w1': nrm((L, NSA_CMP_BLOCK * HEAD_DIM, NSA_CMP_HIDDEN), (NSA_CMP_BLOCK * HEAD_DIM) ** -0.5),
        'cmp_v_w2': nrm((L, NSA_CMP_HIDDEN, HEAD_DIM), NSA_CMP_HIDDEN ** -0.5),
        'cmp_v_pe': nrm((L, NSA_CMP_BLOCK, HEAD_DIM), 0.1),
        'dsa_kv_norm': 1.0 + nrm((L, DSA_KV_RANK), 0.01),
        'dsa_w_uk': nrm((L, DSA_KV_RANK, HEAD_DIM), DSA_KV_RANK ** -0.5),
        'dsa_w_uv': nrm((L, DSA_KV_RANK, HEAD_DIM), DSA_KV_RANK ** -0.5),
        'rel_bias_table': nrm((REL_BUCKETS, NSA_HEADS + DSA_HEADS), 0.5),
        'ffn_w_gate': nrm((L, D_MODEL, D_FF), D_MODEL ** -0.5),
        'ffn_w_up': nrm((L, D_MODEL, D_FF), D_MODEL ** -0.5),
        'ffn_w_down': nrm((L, D_FF, D_MODEL), D_FF ** -0.5),
        'ffn_conv_w': nrm((L, CONV_WIDTH, D_FF), CONV_WIDTH ** -0.5),
        'ffn_conv_b': nrm((L, D_FF), 0.01),
        'ple_w_gate': nrm((L, D_MODEL, D_MODEL), D_MODEL ** -0.5),
        'ple_w_proj': nrm((L, PLE_DIM, D_MODEL), PLE_DIM ** -0.5),
    }


def reference(x, p, w_in, norm_mix, norm_ffn, norm_ple, norm_final, w_proj_a, w_proj_b, w_proj_c,
              w_out, cmp_k_w1, cmp_k_w2, cmp_k_pe, cmp_v_w1, cmp_v_w2, cmp_v_pe, dsa_kv_norm,
              dsa_w_uk, dsa_w_uv, rel_bias_table, ffn_w_gate, ffn_w_up, ffn_w_down, ffn_conv_w,
              ffn_conv_b, ple_w_gate, ple_w_proj):
    for i in range(DEPTH):
        h = rmsnorm(x, norm_mix[i])
        x = x + token_mixer(h, w_in[i], w_proj_a[i], w_proj_b[i], w_proj_c[i], w_out[i],
                            cmp_k_w1[i], cmp_k_w2[i], cmp_k_pe[i], cmp_v_w1[i], cmp_v_w2[i], cmp_v_pe[i],
                            dsa_kv_norm[i], dsa_w_uk[i], dsa_w_uv[i], rel_bias_table)
        h = rmsnorm(x, norm_ffn[i])
        x = x + conv_ffn(h, ffn_w_gate[i], ffn_w_up[i], ffn_w_down[i], ffn_conv_w[i], ffn_conv_b[i])
        x = x + jax.nn.sigmoid(rmsnorm(x, norm_ple[i]) @ ple_w_gate[i]) * (p[i] @ ple_w_proj[i])
    return rmsnorm(x, norm_final)
```

```python
import math
import numpy as np
from contextlib import ExitStack
import ml_dtypes
import concourse.bass as bass
import concourse.mybir as mybir
from concourse.bass_utils import run_bass_kernel_spmd

F32 = mybir.dt.float32
BF16 = mybir.dt.bfloat16
AF = mybir.ActivationFunctionType
ALU = mybir.AluOpType
AX = mybir.AxisListType
NPBF = ml_dtypes.bfloat16

SAME_ENGINE_SYNC = True
S = 4096
D = 2048
T = 2048
INC = 11616
DFF = 5632
NFC = DFF // 128
EPS = 1e-6
SCALE = 128 ** -0.5
NEG = -30000.0


class Buf:
    __slots__ = ("t", "w", "r", "dsem", "dcount", "name")

    def __init__(self, t, name):
        self.t = t
        self.name = name
        self.w = {}
        self.r = {}
        self.dsem = None
        self.dcount = 0

    def __getitem__(self, idx):
        return self.t[idx]


class KB:
    ENG = ("pe", "act", "dve", "pool", "sp")

    def __init__(self):
        self.nc = bass.Bass("TRN2", target_bir_lowering=False)
        self.es = ExitStack()
        self.ops = {e: [] for e in self.ENG}
        self.cnt = {e: 0 for e in self.ENG}
        self.seen = {e: {} for e in self.ENG}
        self.sems = {}
        self.dma_latest = {}
        self.free_sems = []
        self.phase_owned = []
        self.phase = None
        self.nsem = 0
        self.uid = 0
        for e in ("pe", "act", "dve", "pool"):
            self.sems[e] = self.es.enter_context(self.nc.semaphore("s_" + e))

    def dram_in(self, name, shape, dt):
        return self.nc.dram_tensor(name, list(shape), dt, kind="ExternalInput").ap()

    def dram_out(self, name, shape, dt):
        return self.nc.dram_tensor(name, list(shape), dt, kind="ExternalOutput").ap()

    def dram_tmp(self, name, shape, dt):
        return self.nc.dram_tensor(name, list(shape), dt).ap()

    def _nm(self, name):
        self.uid += 1
        return "%s_%d" % (name, self.uid)

    def sb(self, name, shape, dt, stack=None, persist=False):
        st = stack or (self.es if (persist or self.phase is None) else self.phase)
        nm = self._nm(name)
        t = st.enter_context(self.nc.sbuf_tensor(nm, list(shape), dt))
        return Buf(t, nm)

    def ps(self, name, shape, dt=F32, stack=None, persist=False):
        st = stack or (self.es if (persist or self.phase is None) else self.phase)
        nm = self._nm(name)
        t = st.enter_context(self.nc.psum_tensor(nm, list(shape), dt))
        return Buf(t, nm)

    def begin(self):
        assert self.phase is None
        self.phase = ExitStack()
        self.phase_owned = []

    def end(self):
        self.barrier(full=True)
        for b in self.phase_owned:
            self.free_sems.append((b.dsem, b.dcount))
            b.dsem = None
        self.phase_owned = []
        self.phase.close()
        self.phase = None

    def _deps(self, reads, writes):
        d = {}
        raw = {}
        for b in reads:
            for k, v in b.w.items():
                if d.get(k, 0) < v:
                    d[k] = v
                if raw.get(k, 0) < v:
                    raw[k] = v
        for b in writes:
            for k, v in b.w.items():
                if d.get(k, 0) < v:
                    d[k] = v
            for k, v in b.r.items():
                if d.get(k, 0) < v:
                    d[k] = v
        self._raw = raw
        return d

    def _emit_waits(self, e, deps):
        seen = self.seen[e]
        for k, v in deps.items():
            if k == e:
                if not SAME_ENGINE_SYNC or e == "pe":
                    continue
                v = self._raw.get(k, 0)
                if v == 0:
                    continue
            if seen.get(k, 0) >= v:
                continue
            seen[k] = v
            sem = self.sems[k]
            self.ops[e].append(lambda eng, s=sem, vv=v: eng.wait_ge(s, vv))

    def _commit(self, tok_key, tok_val, reads, writes):
        for b in reads:
            if b.r.get(tok_key, 0) < tok_val:
                b.r[tok_key] = tok_val
        for b in writes:
            b.w[tok_key] = tok_val
            b.r = {}

    def op(self, e, fn, reads=(), writes=()):
        deps = self._deps(reads, writes)
        self._emit_waits(e, deps)
        self.cnt[e] += 1
        sem = self.sems[e]
        self.ops[e].append(lambda eng, f=fn, s=sem: f(eng).then_inc(s, 1))
        self._commit(e, self.cnt[e], reads, writes)

    def dma(self, q, out_ap, in_ap, reads=(), writes=(), **kw):
        owner = writes[0] if len(writes) else reads[0]
        if owner.dsem is None:
            if self.free_sems:
                key, base = self.free_sems.pop()
            else:
                key = "d%d" % self.nsem
                self.nsem += 1
                self.sems[key] = self.es.enter_context(self.nc.semaphore("s_" + key))
                base = 0
            owner.dsem = key
            owner.dcount = base
            self.phase_owned.append(owner)
        deps = self._deps(reads, writes)
        self._emit_waits(q, deps)
        owner.dcount += 16
        sem = self.sems[owner.dsem]
        self.ops[q].append(lambda eng, o=out_ap, i=in_ap, s=sem, k=kw: eng.dma_start(out=o, in_=i, **k).then_inc(s, 16))
        self._commit(owner.dsem, owner.dcount, reads, writes)
        self.dma_latest[owner.dsem] = owner.dcount

    def barrier(self, full=False):
        allv = {e: self.cnt[e] for e in ("pe", "act", "dve", "pool")}
        if full:
            allv.update(self.dma_latest)
        for e in self.ENG:
            self._emit_waits(e, dict(allv))

    def finish(self):
        self.barrier(full=True)
        nc = self.nc
        ops = self.ops
        with nc.Block() as block:
            @block.tensor
            def _(eng):
                for f in ops["pe"]:
                    f(eng)

            @block.scalar
            def _(eng):
                for f in ops["act"]:
                    f(eng)

            @block.vector
            def _(eng):
                for f in ops["dve"]:
                    f(eng)

            @block.gpsimd
            def _(eng):
                for f in ops["pool"]:
                    f(eng)

            @block.sync
            def _(eng):
                for f in ops["sp"]:
                    f(eng)
        self.es.close()
        return nc

    def stats(self):
        return {e: len(self.ops[e]) for e in self.ENG}


NEGB = -30000.0

def t5_bucket_np(dist):
    n = np.maximum(dist, 0)
    nf = np.maximum(n, 1).astype(np.float32)
    large = 16 + (np.log(nf / np.float32(16)) / np.float32(math.log(128 / 16)) * np.float32(16)).astype(np.int32)
    large = np.minimum(large, 31)
    return np.where(n < 16, n, large)

def near_tiles(tabcol):
    s = np.arange(128)[:, None]; t = np.arange(128)[None, :]
    d0 = t - s
    diag = np.where(d0 >= 0, tabcol[t5_bucket_np(d0)], np.float32(NEGB)).astype(np.float32)
    off1 = tabcol[t5_bucket_np(d0 + 128)].astype(np.float32)
    c31 = np.full((128, 128), tabcol[31], np.float32)
    negt = np.full((128, 128), NEGB, np.float32)
    return diag, off1, c31, negt

def dsa_consts(tab, g):
    db = np.zeros((128, 4, 3, 128), np.float32)
    for h in range(4):
        diag, off1, c31, negt = near_tiles(tab[:, 8 + h])
        tiles = (off1, diag, negt) if g == 0 else (c31, off1, diag)
        for i in range(3):
            db[:, h, i, :] = tiles[i]
    dc31 = np.broadcast_to(tab[31, 8:12][None, :], (128, 4)).astype(np.float32).copy()
    t = np.arange(128)[:, None]; s = np.arange(128)[None, :]
    tri = np.where(s <= t, 0.0, -1e30).astype(np.float32)
    if g == 0:
        caus = np.concatenate([tri, np.full((128, 128), -1e30, np.float32)], 1)
    else:
        caus = np.concatenate([np.zeros((128, 128), np.float32), tri], 1)
    return db, dc31, caus

def nsa_consts(tab, g):
    wslc = np.zeros((128, 4, 8, 128), np.float32)
    wwin = np.zeros((128, 4, 11, 128), np.float32)
    mext = np.zeros((128, 4, 503), np.float32)
    s = np.arange(128)[:, None]; t = np.arange(128)[None, :]
    for hh in range(4):
        col = tab[:, g * 4 + hh]
        diag, off1, c31, negt = near_tiles(col)
        wedge = np.where(t < s, col[31], np.float32(NEGB)).astype(np.float32)
        for i, tl in enumerate([negt, negt, negt, diag, off1, c31, c31, c31]):
            wslc[:, hh, i, :] = tl
        for i, tl in enumerate([negt, negt, negt, diag, off1, c31, c31, wedge, negt, negt, negt]):
            wwin[:, hh, i, :] = tl
        i_ = np.arange(128)[:, None]; m_ = np.arange(503)[None, :]
        dist = i_ - 16 * (m_ - 248) - 31
        mext[:, hh, :] = np.where(dist >= 0, col[t5_bucket_np(dist)], np.float32(NEGB))
    c31v = np.broadcast_to(tab[31, g * 4:g * 4 + 4][None, :], (128, 4)).astype(np.float32).copy()
    keep = np.zeros((128, 128), np.float32); add = np.zeros((128, 128), np.float32)
    for i in range(128):
        for m in range(128):
            r = m - 64
            if r >= 2: kp, ad = 0.0, -1.0
            elif r == 1: kp, ad = (0.0, 2e9) if i >= 64 else (0.0, -1.0)
            elif r == 0: kp, ad = 0.0, 1e9
            elif r == -1: kp, ad = (0.0, 2e9) if i < 64 else (1.0, 0.0)
            else: kp, ad = 1.0, 0.0
            keep[i, m] = kp; add[i, m] = ad
    c = np.arange(256)[:, None]; j = np.arange(64)[None, :]
    ov = ((16 * c < 64 * j + 64) & (16 * c + 32 > 64 * j) & (c < 255)).astype(np.float32)
    ov = ov.reshape(2, 128, 64).transpose(1, 0, 2).copy()
    jj = np.arange(64)[:, None]; ss = np.arange(4096)[None, :]
    eexp = (ss // 64 == jj).astype(np.float32)
    return dict(wslc=wslc.reshape(128, 4, 1024), wwin=wwin.reshape(128, 4, 1408), mext=mext, nc31=c31v, keep=keep, add=add, ov=ov, eexp=eexp)


def dsa_consts_exact(tab):
    db = np.zeros((128, 4, 2, 128), np.float32)
    for h in range(4):
        diag, off1, c31, negt = near_tiles(tab[:, 8 + h])
        db[:, h, 0, :] = off1
        db[:, h, 1, :] = diag
    dc31 = np.broadcast_to(tab[31, 8:12][None, :], (128, 4)).astype(np.float32).copy()
    t = np.arange(128)[:, None]; s = np.arange(128)[None, :]
    tri = np.where(s <= t, 0.0, -1e30).astype(np.float32)
    return db, dc31, tri


SEGS = [
    ("sgT", 0, 6144, "FM", F32, "sig"),
    ("qnT", 6144, 1024, "FM", BF16, None),
    ("kcT", 7168, 256, "FM", BF16, None),
    ("vcT", 7424, 256, "FM", BF16, None),
    ("ksT", 7680, 256, "FM", BF16, None),
    ("vs", 7936, 256, "TM", BF16, None),
    ("kwT", 8192, 256, "FM", BF16, None),
    ("vw", 8448, 256, "TM", BF16, None),
    ("ng", 8704, 24, "TM", F32, None),
    ("sbqT", 8728, 512, "FM", BF16, None),
    ("sbkT", 9240, 512, "FM", BF16, None),
    ("sbv", 9752, 512, "TM", BF16, None),
    ("dqT", 10264, 512, "FM", BF16, None),
    ("ckv", 10776, 256, "CKV", None, None),
    ("iqT", 11032, 512, "FM", BF16, None),
    ("ikT", 11544, 64, "FM", BF16, None),
    ("iw", 11608, 8, "TM", F32, None),
]


def make_identity(k, name="ident"):
    idf = k.sb(name + "_f", [128, 128], F32, persist=True)
    idn = k.sb(name, [128, 128], BF16, persist=True)
    k.op("pool", lambda e: e.memset(idf[:], 0.0), writes=[idf])
    k.op("pool", lambda e: e.affine_select(out=idf[:], in_=idf[:], pattern=[[-1, 128]], compare_op=ALU.not_equal,
                                           fill=1.0, base=0, channel_multiplier=1), reads=[idf], writes=[idf])
    k.op("dve", lambda e: e.tensor_copy(idn[:], idf[:]), reads=[idf], writes=[idn])
    return idn, idf


def rmsnorm_to_hT(k, x_ap, gb, hT, ident, ntiles, tcol0=0, nfeat=D, tag="n", bufs=None):
    if bufs is None:
        bufs = {}
    if "xt" not in bufs:
        bufs["xt"] = [k.sb("%s_xt%d" % (tag, i), [128, nfeat], F32) for i in range(2)]
        bufs["hb"] = [k.sb("%s_hb%d" % (tag, i), [128, nfeat], BF16) for i in range(2)]
        bufs["junk"] = k.sb(tag + "_junk", [128, nfeat], BF16)
        bufs["ss"] = [k.sb("%s_ss%d" % (tag, i), [128, 1], F32) for i in range(2)]
        bufs["rs"] = [k.sb("%s_rs%d" % (tag, i), [128, 1], F32) for i in range(2)]
        bufs["ptr"] = [k.ps("%s_ptr%d" % (tag, i), [128, 4, 128], BF16) for i in range(2)]
    xt, hb, junk, ss, rs, ptr = bufs["xt"], bufs["hb"], bufs["junk"], bufs["ss"], bufs["rs"], bufs["ptr"]
    nkc = nfeat // 128
    for t in range(ntiles):
        x_, h_, s_, r_ = xt[t % 2], hb[t % 2], ss[t % 2], rs[t % 2]
        k.dma("sp", x_[:], x_ap[t * 128:(t + 1) * 128, :], writes=[x_])
        k.op("act", lambda e, x_=x_, s_=s_: e.activation(out=junk[:], in_=x_[:], func=AF.Square, accum_out=s_[:]),
             reads=[x_], writes=[junk, s_])
        k.op("dve", lambda e, s_=s_, r_=r_: e.tensor_scalar(r_[:], s_[:], 1.0 / nfeat, EPS, op0=ALU.mult, op1=ALU.add),
             reads=[s_], writes=[r_])
        k.op("act", lambda e, r_=r_: e.activation(out=r_[:], in_=r_[:], func=AF.Sqrt), reads=[r_], writes=[r_])
        k.op("dve", lambda e, r_=r_: e.reciprocal(r_[:], r_[:]), reads=[r_], writes=[r_])
        k.op("dve", lambda e, x_=x_, h_=h_, r_=r_: e.scalar_tensor_tensor(out=h_[:], in0=x_[:], scalar=r_[:, 0:1], in1=gb[:],
                                                                          op0=ALU.mult, op1=ALU.mult),
             reads=[x_, r_, gb], writes=[h_])
        for q in range(nkc // 4):
            p_ = ptr[(t * (nkc // 4) + q) % 2]
            for j in range(4):
                kc = q * 4 + j
                k.op("pe", lambda e, p_=p_, h_=h_, kc=kc, j=j: e.transpose(p_[:, j, :], h_[:, kc * 128:(kc + 1) * 128], ident[:]),
                     reads=[h_, ident], writes=[p_])
            eng = "act" if q % 2 == 0 else "dve"
            dst = hT[:, q * 4:(q + 1) * 4, tcol0 + t * 128: tcol0 + (t + 1) * 128]
            if eng == "act":
                k.op("act", lambda e, dst=dst, p_=p_: e.copy(dst, p_[:]), reads=[p_], writes=[hT])
            else:
                k.op("dve", lambda e, dst=dst, p_=p_: e.tensor_copy(dst, p_[:]), reads=[p_], writes=[hT])


def wload(k, buf, pieces, c0, n):
    kc0 = 0
    for (w_ap, r0, nkc) in pieces:
        src = w_ap[r0:r0 + nkc * 128, c0:c0 + n].rearrange("(kc p) c -> p kc c", p=128)
        k.dma("pool", buf[:, kc0:kc0 + nkc, 0:n], src, writes=[buf])
        kc0 += nkc


def p1_body(k, c, x, w_in, gb_d, kvg_d, wuk_d, wuv_d, outs, tok):
    ident = c["ident"]
    gb = k.sb("gb_s", [128, D], F32)
    k.dma("sp", gb[:], gb_d[:, :], writes=[gb])
    kvg = k.sb("kvg_s", [128, 256], F32)
    k.dma("sp", kvg[:], kvg_d[:, :], writes=[kvg])
    hTb = [k.sb("hT%d" % i, [128, 16, 512], BF16) for i in range(T // 512)]
    nbufs = {}
    for tb_ in range(T // 512):
        rmsnorm_to_hT(k, x[tok + tb_ * 512: tok + (tb_ + 1) * 512, :], gb, hTb[tb_], ident, 4, bufs=nbufs)

    wbufs = [k.sb("win_wb%d" % i, [128, 16, 256], BF16) for i in range(3)]
    wcnt = [0]

    def wl_load(c0, n):
        b_ = wbufs[wcnt[0] % 3]
        wcnt[0] += 1
        wload(k, b_, [(w_in, 0, 16)], c0, n)
        return b_
    pacc = [k.ps("pacc%d" % i, [128, 512], F32) for i in range(4)]
    pi = [0]

    def next_ps():
        p = pacc[pi[0] % 4]
        pi[0] += 1
        return p

    fm_st = {F32: [k.sb("fmst_f%d" % i, [128, T], F32) for i in range(2)],
             BF16: [k.sb("fmst_b%d" % i, [128, T], BF16) for i in range(2)]}
    tm_st = {F32: [k.sb("tmst_f%d" % i, [128, 16, 24], F32) for i in range(2)],
             BF16: [k.sb("tmst_b%d" % i, [128, 16, 256], BF16) for i in range(2)]}
    cnt = {"fm": 0, "tm": 0, "ev": 0}

    def evac(dst_ap, dst_buf, p_, src_ap, act=None):
        if act == "sig":
            k.op("act", lambda e: e.activation(out=dst_ap, in_=src_ap, func=AF.Sigmoid), reads=[p_], writes=[dst_buf])
            return
        cnt["ev"] += 1
        if cnt["ev"] % 2 == 0:
            k.op("act", lambda e: e.copy(dst_ap, src_ap), reads=[p_], writes=[dst_buf])
        else:
            k.op("dve", lambda e: e.tensor_copy(dst_ap, src_ap), reads=[p_], writes=[dst_buf])

    def fm_group(name, c0, n, dt, act, row0):
        wb = wl_load(c0, n)
        M = min(128, n)
        for ch in range(n // M):
            st = fm_st[dt][cnt["fm"] % 2]
            cnt["fm"] += 1
            for tb in range(T // 512):
                p_ = next_ps()
                for kc in range(16):
                    k.op("pe", lambda e, p_=p_, kc=kc, ch=ch, tb=tb, wb=wb: e.matmul(
                        p_[0:M, :], lhsT=wb[:, kc, ch * M:(ch + 1) * M], rhs=hTb[tb][:, kc, :],
                        start=(kc == 0), stop=(kc == 15)), reads=[wb, hTb[tb]], writes=[p_])
                evac(st[0:M, tb * 512:(tb + 1) * 512], st, p_, p_[0:M, :], act)
            k.dma("sp", outs[name][row0 + ch * M: row0 + (ch + 1) * M, tok:tok + T], st[0:M, :], reads=[st])

    def tm_group(name, c0, n, dt, col0):
        wb = wl_load(c0, n)
        st = tm_st[dt][cnt["tm"] % 2]
        cnt["tm"] += 1
        for t in range(T // 128):
            p_ = next_ps()
            for kc in range(16):
                k.op("pe", lambda e, p_=p_, kc=kc, t=t, wb=wb: e.matmul(
                    p_[:, 0:n], lhsT=hTb[t // 4][:, kc, (t % 4) * 128:(t % 4 + 1) * 128], rhs=wb[:, kc, 0:n],
                    start=(kc == 0), stop=(kc == 15)), reads=[wb, hTb[t // 4]], writes=[p_])
            evac(st[:, t, 0:n], st, p_, p_[:, 0:n])
        dst = outs[name][tok:tok + T, col0:col0 + n].rearrange("(t p) c -> p t c", p=128)
        k.dma("sp", dst, st[:, :, 0:n], reads=[st])

    def ckv_group(c0):
        wb = wl_load(c0, 256)
        wuk_s = k.sb("wuk_s", [128, 2, 128], F32)
        wuv_s = k.sb("wuv_s", [128, 2, 128], F32)
        wuk = k.sb("wuk_b", [128, 2, 128], BF16)
        wuv = k.sb("wuv_b", [128, 2, 128], BF16)
        k.dma("sp", wuk_s[:], wuk_d.rearrange("(kc p) c -> p kc c", p=128), writes=[wuk_s])
        k.dma("sp", wuv_s[:], wuv_d.rearrange("(kc p) c -> p kc c", p=128), writes=[wuv_s])
        k.op("pool", lambda e: e.tensor_copy(wuk[:], wuk_s[:]), reads=[wuk_s], writes=[wuk])
        k.op("pool", lambda e: e.tensor_copy(wuv[:], wuv_s[:]), reads=[wuv_s], writes=[wuv])
        cT = k.sb("ckvT", [128, 2, T], BF16)
        cf = [k.sb("ckv_f%d" % i, [128, 256], F32) for i in range(2)]
        cb = [k.sb("ckv_b%d" % i, [128, 256], BF16) for i in range(2)]
        cj = k.sb("ckv_j", [128, 256], BF16)
        css = [k.sb("ckv_ss%d" % i, [128, 1], F32) for i in range(2)]
        ptr = k.ps("ckv_ptr", [128, 2, 128], BF16)
        for t in range(T // 128):
            p_ = next_ps()
            f_, b_, s_ = cf[t % 2], cb[t % 2], css[t % 2]
            for kc in range(16):
                k.op("pe", lambda e, p_=p_, kc=kc, t=t: e.matmul(
                    p_[:, 0:256], lhsT=hTb[t // 4][:, kc, (t % 4) * 128:(t % 4 + 1) * 128], rhs=wb[:, kc, 0:256],
                    start=(kc == 0), stop=(kc == 15)), reads=[wb, hTb[t // 4]], writes=[p_])
            k.op("act", lambda e, p_=p_, f_=f_: e.copy(f_[:], p_[:, 0:256]), reads=[p_], writes=[f_])
            k.op("act", lambda e, f_=f_, s_=s_: e.activation(out=cj[:], in_=f_[:], func=AF.Square, accum_out=s_[:]),
                 reads=[f_], writes=[cj, s_])
            k.op("dve", lambda e, s_=s_: e.tensor_scalar(s_[:], s_[:], 1.0 / 256, EPS, op0=ALU.mult, op1=ALU.add),
                 reads=[s_], writes=[s_])
            k.op("act", lambda e, s_=s_: e.activation(out=s_[:], in_=s_[:], func=AF.Sqrt), reads=[s_], writes=[s_])
            k.op("dve", lambda e, s_=s_: e.reciprocal(s_[:], s_[:]), reads=[s_], writes=[s_])
            k.op("dve", lambda e, f_=f_, b_=b_, s_=s_: e.scalar_tensor_tensor(out=b_[:], in0=f_[:], scalar=s_[:, 0:1], in1=kvg[:],
                                                                              op0=ALU.mult, op1=ALU.mult),
                 reads=[f_, s_, kvg], writes=[b_])
            for j in range(2):
                k.op("pe", lambda e, b_=b_, j=j: e.transpose(ptr[:, j, :], b_[:, j * 128:(j + 1) * 128], ident[:]),
                     reads=[b_, ident], writes=[ptr])
            k.op("dve", lambda e, t=t: e.tensor_copy(cT[:, :, t * 128:(t + 1) * 128], ptr[:]), reads=[ptr], writes=[cT])
        st = fm_st[BF16][cnt["fm"] % 2]
        cnt["fm"] += 1
        for tb in range(T // 512):
            p_ = next_ps()
            for kc in range(2):
                k.op("pe", lambda e, p_=p_, kc=kc, tb=tb: e.matmul(p_[:, :], lhsT=wuk[:, kc, :], rhs=cT[:, kc, tb * 512:(tb + 1) * 512],
                                                                   start=(kc == 0), stop=(kc == 1)), reads=[wuk, cT], writes=[p_])
            evac(st[:, tb * 512:(tb + 1) * 512], st, p_, p_[:, :])
        k.dma("sp", outs["dkT"][:, tok:tok + T], st[:, :], reads=[st])
        st = tm_st[BF16][cnt["tm"] % 2]
        cnt["tm"] += 1
        for t in range(T // 128):
            p_ = next_ps()
            for kc in range(2):
                k.op("pe", lambda e, p_=p_, kc=kc, t=t: e.matmul(p_[:, 0:128], lhsT=cT[:, kc, t * 128:(t + 1) * 128], rhs=wuv[:, kc, :],
                                                                 start=(kc == 0), stop=(kc == 1)), reads=[wuv, cT], writes=[p_])
            evac(st[:, t, 0:128], st, p_, p_[:, 0:128])
        k.dma("sp", outs["dv"][tok:tok + T, :].rearrange("(t p) c -> p t c", p=128), st[:, :, 0:128], reads=[st])

    for (name, c0, n, mode, dt, act) in SEGS:
        if mode == "FM":
            for g0 in range(0, n, 256):
                gn = min(256, n - g0)
                fm_group(name, c0 + g0, gn, dt, act, g0)
        elif mode == "TM":
            for g0 in range(0, n, 256):
                gn = min(256, n - g0)
                tm_group(name, c0 + g0, gn, dt, g0)
        else:
            ckv_group(c0)


P1_OUT = [s[0] for s in SEGS if s[3] != "CKV"] + ["dkT", "dv"]


def consts_common(k):
    c = {}
    c["ident"], c["identf"] = make_identity(k)
    ones = k.sb("ones_bf", [128, 128], BF16, persist=True)
    k.op("pool", lambda e: e.memset(ones[:], 1.0), writes=[ones])
    c["ones"] = ones
    zeros = k.sb("zeros_bf", [128, 512], BF16, persist=True)
    k.op("pool", lambda e: e.memset(zeros[:], 0.0), writes=[zeros])
    c["zeros"] = zeros
    tf = k.sb("tri_f", [128, 128], F32, persist=True)
    tri = k.sb("triS", [128, 128], BF16, persist=True)
    k.op("pool", lambda e: e.memset(tf[:], 1.0), writes=[tf])
    k.op("pool", lambda e: e.affine_select(out=tf[:], in_=tf[:], pattern=[[-1, 128]], compare_op=ALU.is_gt, fill=0.0,
                                           base=0, channel_multiplier=1), reads=[tf], writes=[tf])
    k.op("dve", lambda e: e.tensor_copy(tri[:], tf[:]), reads=[tf], writes=[tri])
    c["triS"] = tri
    return c


def run_pipeline(stage_lists):
    n = len(stage_lists)
    if n == 0:
        return
    nst = max(len(x) for x in stage_lists)
    for it in range(n + nst - 1):
        for s_ in range(nst):
            b = it - s_
            if 0 <= b < n and s_ < len(stage_lists[b]) and stage_lists[b][s_] is not None:
                stage_lists[b][s_]()


def build_sb(k, c, qT_d, kT_d, v_d, oT_d, nheads=2):
    ident, ones, tri, zeros = c["ident"], c["ones"], c["triS"], c["zeros"]
    NS = 3
    qT = [k.sb("sb_qT%d" % i, [128, S], BF16) for i in range(2)]
    kT = [k.sb("sb_kT%d" % i, [128, S], BF16) for i in range(2)]
    V = [k.sb("sb_V%d" % i, [128, 32, 128], BF16) for i in range(2)]
    zp = [k.ps("sb_zp%d" % i, [128, 512], F32) for i in range(2)]
    atri = [k.ps("sb_atri%d" % i, [128, 512], F32) for i in range(2)]
    accp = k.ps("sb_accp", [128, 512], F32)
    po = [k.ps("sb_po%d" % i, [128, 4, 128], F32) for i in range(2)]
    ptr = k.ps("sb_ptr", [128, 4, 128], BF16)
    E = [k.sb("sb_E%d" % i, [128, 512], F32) for i in range(NS)]
    SPb = [k.sb("sb_SP%d" % i, [128, 512], BF16) for i in range(NS + 1)]
    T1 = [k.sb("sb_T1%d" % i, [128, 512], F32) for i in range(NS)]
    T2 = [k.sb("sb_T2%d" % i, [128, 512], F32) for i in range(NS)]
    T3 = [k.sb("sb_T3%d" % i, [128, 512], F32) for i in range(NS)]
    W = [k.sb("sb_W%d" % i, [128, 512], BF16) for i in range(NS)]
    osb = [k.sb("sb_osb%d" % i, [128, 4, 128], BF16) for i in range(2)]
    oTs = [k.sb("sb_oTs%d" % i, [128, 512], BF16) for i in range(2)]
    stages = []
    it = 0
    for h in range(nheads):
        qT_, kT_, V_ = qT[h % 2], kT[h % 2], V[h % 2]
        first_of_head = True
        for qb in range(S // 512):
            po_ = po[qb % 2]
            kts = list(reversed(range(4 * qb + 4)))
            for bi, kt in enumerate(kts):
                kk = kt - 4 * qb
                n0 = max(kk, 0) * 128
                i3, i2 = it % NS, it % 2
                it += 1
                zp_, at_ = zp[i2], atri[i2]
                E_, SP_, T1_, T2_, T3_, W_ = E[i3], SPb[(it - 1) % (NS + 1)], T1[i3], T2[i3], T3[i3], W[i3]
                cs = slice(n0, 512)
                ds = slice(n0, n0 + 128)
                first = (bi == 0)
                last = (bi == len(kts) - 1)
                prev_blk = None if first else prev_info
                prev_info = (SP_, cs)

                def st0(h=h, qb=qb, kt=kt, kk=kk, n0=n0, cs=cs, ds=ds, zp_=zp_, E_=E_, SP_=SP_, qT_=qT_, kT_=kT_, V_=V_, load=first_of_head):
                    if load:
                        k.dma("sp", qT_[:], qT_d[h * 128:(h + 1) * 128, :], writes=[qT_])
                        k.dma("sp", kT_[:], kT_d[h * 128:(h + 1) * 128, :], writes=[kT_])
                        k.dma("sp", V_[:], v_d[:, h * 128:(h + 1) * 128].rearrange("(t p) d -> p t d", p=128), writes=[V_])
                    k.op("pe", lambda e: e.matmul(zp_[:, cs], lhsT=kT_[:, kt * 128:(kt + 1) * 128], rhs=qT_[:, qb * 512 + n0:(qb + 1) * 512], start=True, stop=True),
                         reads=[kT_, qT_], writes=[zp_])
                    k.op("act", lambda e: e.activation(out=E_[:, cs], in_=zp_[:, cs], func=AF.Exp, scale=SCALE), reads=[zp_], writes=[E_])
                    k.op("act", lambda e: e.activation(out=SP_[:, cs], in_=E_[:, cs], func=AF.Ln, bias=1.0), reads=[E_], writes=[SP_])
                    if kk >= 0:
                        k.op("pool", lambda e: e.affine_select(out=SP_[:, ds], in_=SP_[:, ds], pattern=[[1, 128]], compare_op=ALU.is_gt, fill=0.0, base=0,
                                                               channel_multiplier=-1), reads=[SP_], writes=[SP_])

                def st1(kk=kk, cs=cs, ds=ds, zp_=zp_, at_=at_, SP_=SP_, T1_=T1_, T2_=T2_, T3_=T3_, W_=W_, first=first, prev=prev_blk):
                    k.op("pe", lambda e: e.matmul(at_[:, cs], lhsT=tri[:], rhs=SP_[:, cs], start=True, stop=True), reads=[tri, SP_], writes=[at_])
                    if first:
                        k.op("pe", lambda e: e.matmul(accp[:, :], lhsT=zeros[:, 0:128], rhs=zeros[:, :], start=True, stop=False), reads=[zeros], writes=[accp])
                    else:
                        pSP, pcs = prev
                        k.op("pe", lambda e: e.matmul(accp[:, pcs], lhsT=ones[:], rhs=pSP[:, pcs], start=False, stop=False), reads=[ones, pSP], writes=[accp])
                    k.op("dve", lambda e: e.scalar_tensor_tensor(out=T1_[:, cs], in0=zp_[:, cs], scalar=SCALE, in1=SP_[:, cs], op0=ALU.mult, op1=ALU.subtract),
                         reads=[zp_, SP_], writes=[T1_])
                    k.op("dve", lambda e: e.tensor_tensor(out=T2_[:, cs], in0=T1_[:, cs], in1=at_[:, cs], op=ALU.subtract), reads=[T1_, at_], writes=[T2_])
                    k.op("dve", lambda e: e.tensor_tensor(out=T3_[:, cs], in0=T2_[:, cs], in1=accp[:, cs], op=ALU.subtract), reads=[T2_, accp], writes=[T3_])
                    k.op("act", lambda e: e.activation(out=W_[:, cs], in_=T3_[:, cs], func=AF.Exp), reads=[T3_], writes=[W_])
                    if kk >= 0:
                        k.op("pool", lambda e: e.affine_select(out=W_[:, ds], in_=W_[:, ds], pattern=[[1, 128]], compare_op=ALU.is_gt, fill=0.0, base=0,
                                                               channel_multiplier=-1), reads=[W_], writes=[W_])

                def st2(h=h, qb=qb, kt=kt, kk=kk, W_=W_, V_=V_, po_=po_, first=first, last=last):
                    if first:
                        k.op("pe", lambda e: e.matmul(po_[:].rearrange("p a b -> p (a b)"), lhsT=zeros[:, 0:128], rhs=zeros[:, :], start=True, stop=False),
                             reads=[zeros], writes=[po_])
                    for j in range(max(kk, 0), 4):
                        k.op("pe", lambda e, j=j: e.matmul(po_[:, j, :], lhsT=W_[:, j * 128:(j + 1) * 128], rhs=V_[:, kt, :], start=False, stop=(kt == 0 and j == 3)),
                             reads=[W_, V_], writes=[po_])
                    if last:
                        o_ = osb[qb % 2]
                        oT_ = oTs[qb % 2]
                        k.op("act", lambda e: e.copy(o_[:], po_[:]), reads=[po_], writes=[o_])
                        for j in range(4):
                            k.op("pe", lambda e, j=j: e.transpose(ptr[:, j, :], o_[:, j, :], ident[:]), reads=[o_, ident], writes=[ptr])
                        k.op("dve", lambda e: e.tensor_copy(oT_[:], ptr[:].rearrange("p a b -> p (a b)")), reads=[ptr], writes=[oT_])
                        k.dma("pool", oT_d[h * 128:(h + 1) * 128, qb * 512:(qb + 1) * 512], oT_[:], reads=[oT_])

                stages.append([st0, st1, st2])
                first_of_head = False
    run_pipeline(stages)


def build_nsa(k, c, d, oaT_d):
    ident, zeros = c["ident"], c["zeros"]
    A = [k.ps("n_A%d" % i, [128, 512], F32) for i in range(2)]
    B = [k.ps("n_B%d" % i, [128, 512], F32) for i in range(2)]
    Cb = [k.ps("n_C%d" % i, [128, 2, 256], F32) for i in range(2)]
    Dt = k.ps("n_D", [128, 4, 128], BF16)
    qT = k.sb("n_qT", [128, 4, S], BF16)
    k.dma("sp", qT[:], d["qnT"].rearrange("(h e) t -> e h t", h=4), writes=[qT])
    ksT = k.sb("n_ksT", [128, S], BF16)
    kwT = k.sb("n_kwT", [128, S], BF16)
    k.dma("sp", ksT[:], d["ksT"][:, :], writes=[ksT])
    k.dma("sp", kwT[:], d["kwT"][:, :], writes=[kwT])
    V1s = k.sb("n_V1s", [128, 32, 129], BF16)
    V1w = k.sb("n_V1w", [128, 32, 129], BF16)
    for V1, nm in ((V1s, "vs"), (V1w, "vw")):
        k.op("pool", lambda e, V1=V1: e.memset(V1[:, :, 128:129], 1.0), writes=[V1])
        k.dma("sp", V1[:, :, 0:128], d[nm].rearrange("(t p) e -> p t e", p=128), writes=[V1])
    wslc = k.sb("n_wslc", [128, 4, 1024], F32)
    wwin = k.sb("n_wwin", [128, 4, 1408], F32)
    mext = k.sb("n_mext", [128, 4, 503], F32)
    nc31 = k.sb("n_c31", [128, 4], F32)
    keepm = k.sb("n_keep", [128, 128], F32)
    addm = k.sb("n_add", [128, 128], F32)
    ovb = k.sb("n_ov", [128, 2, 64], BF16)
    eexp = k.sb("n_eexp", [64, S], BF16)
    for t_, nm in ((wslc, "wslc"), (wwin, "wwin"), (mext, "mext"), (nc31, "nc31"), (keepm, "keep"), (addm, "add"), (ovb, "ov"), (eexp, "eexp")):
        k.dma("sp", t_[:], d[nm], writes=[t_])
    ng = k.sb("n_ng", [128, 32, 12], F32)
    sg = k.sb("n_sg", [128, 32, 12], F32)
    k.dma("sp", ng[:], d["ng"].rearrange("(t p) c -> p t c", p=128), writes=[ng])
    k.op("act", lambda e: e.activation(out=sg[:], in_=ng[:], func=AF.Sigmoid), reads=[ng], writes=[sg])
    kcTc = k.sb("n_kcTc", [128, 256], BF16)
    vc = k.sb("n_vc", [128, 2, 128], BF16)
    k.op("pool", lambda e: e.memset(vc[:], 0.0), writes=[vc])

    st = ExitStack()
    aT = k.sb("n_aT", [128, S], BF16, stack=st)
    w1s = [k.sb("n_w1s%d" % i, [128, 8, 256], F32, stack=st) for i in range(2)]
    w1b = k.sb("n_w1b", [128, 32, 256], BF16, stack=st)
    w2s = k.sb("n_w2s", [128, 2, 128], F32, stack=st)
    w2b = k.sb("n_w2b", [128, 2, 128], BF16, stack=st)
    pes = k.sb("n_pes", [128, 32], F32, stack=st)
    peb = k.sb("n_peb", [128, 32], BF16, stack=st)
    pbias = k.sb("n_pbias", [128, 2], F32, stack=st)
    gT = k.sb("n_gT", [128, 2, 256], BF16, stack=st)
    k.op("pool", lambda e: e.memset(gT[:], 0.0), writes=[gT])
    for which in ("k", "v"):
        k.dma("sp", aT[:], d["kcT" if which == "k" else "vcT"][:, :], writes=[aT])
        w1_d, w2_d, pe_d = d["c%s_w1" % which], d["c%s_w2" % which], d["c%s_peT" % which]
        for q in range(4):
            s_ = w1s[q % 2]
            k.dma("sp", s_[:], w1_d[q * 1024:(q + 1) * 1024, :].rearrange("(p e) h -> e p h", e=128), writes=[s_])
            k.op("pool", lambda e, s_=s_, q=q: e.tensor_copy(w1b[:, q * 8:(q + 1) * 8, :], s_[:]), reads=[s_], writes=[w1b])
        k.dma("sp", w2s[:], w2_d.rearrange("(c p) e -> p c e", p=128), writes=[w2s])
        k.op("pool", lambda e: e.tensor_copy(w2b[:], w2s[:]), reads=[w2s], writes=[w2b])
        k.dma("sp", pes[:], pe_d[:, :], writes=[pes])
        k.op("pool", lambda e: e.tensor_copy(peb[:], pes[:]), reads=[pes], writes=[peb])
        for hc in range(2):
            pb = B[hc]
            for p in range(32):
                k.op("pe", lambda e, pb=pb, p=p, hc=hc: e.matmul(pb[:, 0:1], lhsT=w1b[:, p, hc * 128:(hc + 1) * 128], rhs=peb[:, p:p + 1],
                                                               start=(p == 0), stop=(p == 31)), reads=[w1b, peb], writes=[pb])
            k.op("dve", lambda e, pb=pb, hc=hc: e.tensor_copy(pbias[:, hc:hc + 1], pb[:, 0:1]), reads=[pb], writes=[pbias])
            ph = A[hc]
            for p in range(32):
                k.op("pe", lambda e, ph=ph, p=p, hc=hc: e.matmul(ph[:, 0:255], lhsT=w1b[:, p, hc * 128:(hc + 1) * 128], rhs=aT[:, p:p + 4065:16],
                                                               start=(p == 0), stop=(p == 31)), reads=[w1b, aT], writes=[ph])
            k.op("act", lambda e, ph=ph, hc=hc: e.activation(out=gT[:, hc, 0:255], in_=ph[:, 0:255], func=AF.Gelu_apprx_tanh, bias=pbias[:, hc:hc + 1]),
                 reads=[ph, pbias], writes=[gT])
        if which == "k":
            po_ = B[0]
            for hc in range(2):
                k.op("pe", lambda e, hc=hc, po_=po_: e.matmul(po_[:, 0:255], lhsT=w2b[:, hc, :], rhs=gT[:, hc, 0:255], start=(hc == 0), stop=(hc == 1)),
                     reads=[w2b, gT], writes=[po_])
            k.op("pool", lambda e: e.memset(kcTc[:], 0.0), writes=[kcTc])
            k.op("act", lambda e, po_=po_: e.copy(kcTc[:, 0:255], po_[:, 0:255]), reads=[po_], writes=[kcTc])
        else:
            for ch in range(2):
                rows = 128 if ch == 0 else 127
                po_ = B[ch]
                for hc in range(2):
                    k.op("pe", lambda e, hc=hc, ch=ch, rows=rows, po_=po_: e.matmul(po_[0:rows, 0:128], lhsT=gT[:, hc, ch * 128:ch * 128 + rows], rhs=w2b[:, hc, :],
                                                                                 start=(hc == 0), stop=(hc == 1)), reads=[w2b, gT], writes=[po_])
                k.op("act", lambda e, ch=ch, rows=rows, po_=po_: e.copy(vc[0:rows, ch, :], po_[0:rows, 0:128]), reads=[po_], writes=[vc])
    k.barrier()
    st.close()

    Lc = k.sb("n_Lc", [128, 4, 256], F32)
    ec = k.sb("n_ec", [128, 4, 256], F32)
    pbf = k.sb("n_pbf", [128, 4, 256], BF16)
    k.op("pool", lambda e: e.memset(pbf[:], 0.0), writes=[pbf])
    pT = k.sb("n_pT", [128, 4, 2, 128], BF16)
    mx = k.sb("n_mx", [128, 4], F32)
    Zc = k.sb("n_Zc", [128, 4], F32)
    scs = k.sb("n_scs", [128, 64], F32)
    swork = k.sb("n_swork", [128, 64], F32)
    m8a = k.sb("n_m8a", [128, 8], F32)
    m8b = k.sb("n_m8b", [128, 8], F32)
    sel = k.sb("n_sel", [128, 64], BF16)
    selT = k.sb("n_selT", [64, 512], BF16)
    Oacc = k.sb("n_Oacc", [128, 4, 4, 128], F32)
    Lg = [k.sb("n_Lg%d" % i, [128, 512], F32) for i in range(3)]
    Eb = [k.sb("n_E%d" % i, [128, 512], F32) for i in range(3)]
    PT = [k.sb("n_PT%d" % i, [128, 512], BF16) for i in range(5)]
    rz = k.sb("n_rz", [128, 1], F32)
    maskS = k.sb("n_maskS", [128, 32, 512], BF16)
    ocp = [k.sb("n_ocp%d" % i, [128, 2, 2, 256], F32) for i in range(1)]
    obf = k.sb("n_obf", [128, 4, 4, 128], BF16)
    oTs = [k.sb("n_oTs%d" % i, [128, 4, 128], BF16) for i in range(2)]
    ci = [0]

    def attn(qb, h, br):
        kT, V1, wide, gcol = (ksT, V1s, wslc, 1) if br == "slc" else (kwT, V1w, wwin, 2)
        for cb in Cb:
            k.op("pe", lambda e, cb=cb: e.matmul(cb[:].rearrange("p a b -> p (a b)"), lhsT=zeros[:, 0:128], rhs=zeros[:, :], start=True, stop=False),
                 reads=[zeros], writes=[cb])
        kts = list(range(0, 4 * qb + 4)) if br == "slc" else list(range(max(0, 4 * qb - 4), 4 * qb + 4))
        stages = []
        for kt in kts:
            kk = kt - 4 * qb
            jlo = max(kk, 0)
            jhi = 3 if br == "slc" else min(3, kk + 4)
            cs = slice(jlo * 128, (jhi + 1) * 128)
            i_ = ci[0] % 2
            i3 = ci[0] % 3
            i5 = ci[0] % 5
            ci[0] += 1
            ps_, Lg_, E_, PT_, pm_ = A[i_], Lg[i3], Eb[i3], PT[i5], B[i_]

            def st0(kt=kt, kk=kk, jlo=jlo, jhi=jhi, cs=cs, ps_=ps_, Lg_=Lg_, E_=E_, PT_=PT_, pm_=pm_):
                k.op("pe", lambda e: e.matmul(ps_[:, cs], lhsT=kT[:, kt * 128:(kt + 1) * 128], rhs=qT[:, h, qb * 512 + jlo * 128: qb * 512 + (jhi + 1) * 128],
                                              start=True, stop=True), reads=[kT, qT], writes=[ps_])
                near = (kk >= -1) if br == "slc" else True
                eout = E_ if br == "slc" else PT_
                if near:
                    w0 = (3 - kk) * 128
                    k.op("dve", lambda e: e.scalar_tensor_tensor(out=Lg_[:, cs], in0=ps_[:, cs], scalar=SCALE, in1=wide[:, h, w0 + jlo * 128: w0 + (jhi + 1) * 128],
                                                                 op0=ALU.mult, op1=ALU.add), reads=[ps_, wide], writes=[Lg_])
                    k.op("act", lambda e: e.activation(out=eout[:, cs], in_=Lg_[:, cs], func=AF.Exp), reads=[Lg_], writes=[eout])
                else:
                    k.op("act", lambda e: e.activation(out=eout[:, cs], in_=ps_[:, cs], func=AF.Exp, scale=SCALE, bias=nc31[:, h:h + 1]), reads=[ps_, nc31], writes=[eout])

            def st0b(cs=cs, E_=E_, PT_=PT_, kt=kt):
                k.op("dve", lambda e: e.tensor_tensor(out=PT_[:, cs], in0=E_[:, cs], in1=maskS[:, kt, cs], op=ALU.mult), reads=[E_, maskS], writes=[PT_])

            def st1(kt=kt, kk=kk, jlo=jlo, jhi=jhi, PT_=PT_):
                for j in range(jlo, jhi + 1):
                    cb = Cb[j // 2]
                    k.op("pe", lambda e, j=j, cb=cb: e.matmul(cb[:, j % 2, 0:129], lhsT=PT_[:, j * 128:(j + 1) * 128], rhs=V1[:, kt, :],
                                                              start=False, stop=(kk == j and j % 2 == 1)), reads=[PT_, V1], writes=[cb])

            stages.append([st0, st0b if br == "slc" else None, None, st1])
        run_pipeline(stages)
        oc_ = ocp[0]
        for b_ in range(2):
            k.op("act", lambda e, b_=b_: e.copy(oc_[:, b_, :, :], Cb[b_][:]), reads=[Cb[b_]], writes=[oc_])
        for j in range(4):
            qt = 4 * qb + j
            k.op("dve", lambda e, j=j: e.reciprocal(rz[:], oc_[:, j // 2, j % 2, 128:129]), reads=[oc_], writes=[rz])
            k.op("dve", lambda e, qt=qt: e.tensor_scalar(rz[:], rz[:], sg[:, qt, 3 * h + gcol:3 * h + gcol + 1], None, op0=ALU.mult), reads=[rz, sg], writes=[rz])
            k.op("dve", lambda e, j=j: e.scalar_tensor_tensor(out=Oacc[:, j, h, :], in0=oc_[:, j // 2, j % 2, 0:128], scalar=rz[:, 0:1], in1=Oacc[:, j, h, :],
                                                             op0=ALU.mult, op1=ALU.add), reads=[oc_, rz, Oacc], writes=[Oacc])

    for qb in range(S // 512):
        for j in range(4):
            qt = 4 * qb + j
            for h in range(4):
                pl = A[h // 2]
                k.op("pe", lambda e, pl=pl, h=h, qt=qt: e.matmul(pl[:, (h % 2) * 256:(h % 2) * 256 + 255], lhsT=qT[:, h, qt * 128:(qt + 1) * 128], rhs=kcTc[:, 0:255],
                                                               start=True, stop=True), reads=[qT, kcTc], writes=[pl])
            for h in range(4):
                pl = A[h // 2]
                k.op("dve", lambda e, pl=pl, h=h, qt=qt: e.scalar_tensor_tensor(out=Lc[:, h, 0:255], in0=pl[:, (h % 2) * 256:(h % 2) * 256 + 255], scalar=SCALE,
                                                                              in1=mext[:, h, 248 - 8 * qt: 503 - 8 * qt], op0=ALU.mult, op1=ALU.add),
                     reads=[pl, mext], writes=[Lc])
            k.op("dve", lambda e: e.tensor_reduce(out=mx[:], in_=Lc[:, :, 0:255], axis=AX.X, op=ALU.max), reads=[Lc], writes=[mx])
            k.op("dve", lambda e: e.tensor_scalar(mx[:], mx[:], -100.0, -1.0, op0=ALU.max, op1=ALU.mult), reads=[mx], writes=[mx])
            for h in range(4):
                k.op("act", lambda e, h=h: e.activation(out=ec[:, h, 0:255], in_=Lc[:, h, 0:255], func=AF.Exp, bias=mx[:, h:h + 1], accum_out=Zc[:, h:h + 1]),
                     reads=[Lc, mx], writes=[ec, Zc])
            k.op("dve", lambda e: e.tensor_scalar(Zc[:], Zc[:], 1e-30, None, op0=ALU.max), reads=[Zc], writes=[Zc])
            k.op("dve", lambda e: e.reciprocal(Zc[:], Zc[:]), reads=[Zc], writes=[Zc])
            for h in range(4):
                eng = "dve" if h % 2 == 0 else "pool"
                k.op(eng, lambda e, h=h: e.tensor_scalar(pbf[:, h, 0:255], ec[:, h, 0:255], Zc[:, h:h + 1], None, op0=ALU.mult), reads=[ec, Zc], writes=[pbf])
            for h in range(4):
                for ch in range(2):
                    k.op("pe", lambda e, h=h, ch=ch: e.transpose(Dt[:, ch, :], pbf[:, h, ch * 128:(ch + 1) * 128], ident[:]), reads=[pbf, ident], writes=[Dt])
                if h % 2 == 0:
                    k.op("act", lambda e, h=h: e.copy(pT[:, h, :, :], Dt[:, 0:2, :]), reads=[Dt], writes=[pT])
                else:
                    k.op("dve", lambda e, h=h: e.tensor_copy(pT[:, h, :, :], Dt[:, 0:2, :]), reads=[Dt], writes=[pT])
            pcmp = B[0]
            pimp = B[1]
            for h in range(4):
                for ch in range(2):
                    k.op("pe", lambda e, h=h, ch=ch: e.matmul(pcmp[:, h * 128:(h + 1) * 128], lhsT=pT[:, h, ch, :], rhs=vc[:, ch, :], start=(ch == 0), stop=(ch == 1)),
                         reads=[pT, vc], writes=[pcmp])
            for h in range(4):
                for ch in range(2):
                    k.op("pe", lambda e, h=h, ch=ch: e.matmul(pimp[:, 0:64], lhsT=pT[:, h, ch, :], rhs=ovb[:, ch, :], start=(h == 0 and ch == 0), stop=(h == 3 and ch == 1)),
                         reads=[pT, ovb], writes=[pimp])
            for h in range(4):
                k.op("dve", lambda e, h=h, j=j, qt=qt: e.tensor_scalar(Oacc[:, j, h, :], pcmp[:, h * 128:(h + 1) * 128], sg[:, qt, 3 * h:3 * h + 1], None, op0=ALU.mult),
                     reads=[pcmp, sg], writes=[Oacc])
            m0 = 64 - 2 * qt
            k.op("dve", lambda e, m0=m0: e.tensor_tensor(out=scs[:], in0=pimp[:, 0:64], in1=keepm[:, m0:m0 + 64], op=ALU.mult), reads=[pimp, keepm], writes=[scs])
            k.op("dve", lambda e, m0=m0: e.tensor_tensor(out=scs[:], in0=scs[:], in1=addm[:, m0:m0 + 64], op=ALU.add), reads=[scs, addm], writes=[scs])
            k.op("dve", lambda e: e.memset(scs[:, 0:1], 3e9), writes=[scs])
            k.op("dve", lambda e: e.max(out=m8a[:], in_=scs[:]), reads=[scs], writes=[m8a])
            k.op("dve", lambda e: e.match_replace(out=swork[:], in_to_replace=m8a[:], in_values=scs[:], imm_value=-2.0), reads=[scs, m8a], writes=[swork])
            k.op("dve", lambda e: e.max(out=m8b[:], in_=swork[:]), reads=[swork], writes=[m8b])
            k.op("dve", lambda e: e.tensor_scalar(sel[:], scs[:], m8b[:, 7:8], None, op0=ALU.is_ge), reads=[scs, m8b], writes=[sel])
            k.op("pe", lambda e: e.transpose(Dt[0:64, 2, :], sel[:], ident[:]), reads=[sel, ident], writes=[Dt])
            k.op("act", lambda e, j=j: e.copy(selT[:, j * 128:(j + 1) * 128], Dt[0:64, 2, :]), reads=[Dt], writes=[selT])
        for kt in range(4 * qb + 4):
            jlo = max(kt - 4 * qb, 0)
            cs = slice(jlo * 128, 512)
            pm_ = B[kt % 2]
            k.op("pe", lambda e, pm_=pm_, kt=kt, cs=cs: e.matmul(pm_[:, cs], lhsT=eexp[:, kt * 128:(kt + 1) * 128], rhs=selT[:, cs], start=True, stop=True),
                 reads=[eexp, selT], writes=[pm_])
            k.op("act", lambda e, pm_=pm_, kt=kt, cs=cs: e.copy(maskS[:, kt, cs], pm_[:, cs]), reads=[pm_], writes=[maskS])
        for h in range(4):
            attn(qb, h, "slc")
            attn(qb, h, "win")
        k.op("act", lambda e: e.copy(obf[:], Oacc[:]), reads=[Oacc], writes=[obf])
        for h in range(4):
            oT_ = oTs[h % 2]
            for j in range(4):
                k.op("pe", lambda e, h=h, j=j: e.transpose(Dt[:, j, :], obf[:, j, h, :], ident[:]), reads=[obf, ident], writes=[Dt])
            k.op("dve", lambda e, oT_=oT_: e.tensor_copy(oT_[:], Dt[:]), reads=[Dt], writes=[oT_])
            k.dma("pool", oaT_d[h * 128:(h + 1) * 128, qb * 512:(qb + 1) * 512], oT_[:].rearrange("p a b -> p (a b)"), reads=[oT_])


def build_dsa(k, c, dqT_d, dkT_d, dv_d, iqT_d, ikT_d, iw_d, bias_d, c31_d, caus_d, ocT_d, nqt=16, near=3, parity=True):
    ident = c["ident"]
    WSC = (8 ** -0.5) * (64 ** -0.5)
    CW = (near - 1) * 128
    dkT = k.sb("d_kT", [128, S], BF16)
    V1 = k.sb("d_V1", [128, 32, 129], BF16)
    ikT = k.sb("d_ikT", [64, S], BF16)
    bias = k.sb("d_bias", [128, 4, near, 128], F32)
    caus = k.sb("d_caus", [128, CW], F32)
    c31 = k.sb("d_c31", [128, 4], F32)
    k.dma("sp", caus[:], caus_d[:, :], writes=[caus])
    k.dma("sp", dkT[:], dkT_d[:, :], writes=[dkT])
    k.op("pool", lambda e: e.memset(V1[:, :, 128:129], 1.0), writes=[V1])
    k.dma("sp", V1[:, :, 0:128], dv_d.rearrange("(t p) d -> p t d", p=128), writes=[V1])
    k.dma("sp", ikT[:], ikT_d[:, :], writes=[ikT])
    k.dma("sp", bias[:], bias_d[:, :, :, :], writes=[bias])
    k.dma("sp", c31[:], c31_d[:, :], writes=[c31])
    dq = [k.sb("d_q%d" % i, [128, 4, 128], BF16) for i in range(2)]
    iq = [k.sb("d_iq%d" % i, [64, 8, 128], BF16) for i in range(2)]
    iw = [k.sb("d_iw%d" % i, [128, 8], F32) for i in range(2)]
    absw = [k.sb("d_absw%d" % i, [128, 8], F32) for i in range(2)]
    sgn = [k.sb("d_sgn%d" % i, [128, 8], F32) for i in range(2)]
    sc = [k.sb("d_sc%d" % i, [128, S], F32) for i in range(2)]
    jk1 = [k.sb("d_jk1%d" % i, [128, S], BF16) for i in range(2)]
    jk2 = [k.sb("d_jk2%d" % i, [128, S], BF16) for i in range(2)]
    sv = [{nm: k.sb("d_%s%d" % (nm, i), [128, 2 if nm == "cnt2" else 1], F32) for nm in ("lo", "hi", "w0", "t1", "nt2", "cnt2", "asum")} for i in range(2)]
    mask = k.sb("d_mask", [128, S], BF16)
    maskT = k.sb("d_maskT", [128, 32, 128], BF16)
    R = [k.sb("d_R%d" % i, [128, 512], F32) for i in range(3)]
    E = [k.sb("d_E%d" % i, [128, 4, 128], F32) for i in range(3)]
    Lg = [k.sb("d_Lg%d" % i, [128, 128], F32) for i in range(3)]
    PT = [k.sb("d_PT%d" % i, [128, 4, 128], BF16) for i in range(4)]
    rz = k.sb("d_rz", [128, 1], F32)
    osb = [k.sb("d_osb%d" % i, [128, 4, 128], BF16) for i in range(2)]
    oTs = [k.sb("d_oTs%d" % i, [128, 4, 128], BF16) for i in range(2)]
    pdot = [k.ps("d_pdot%d" % i, [128, 512], F32) for i in range(2)]
    pst = [k.ps("d_pst%d" % i, [128, 4, 128], F32) for i in range(2)]
    po = [k.ps("d_po%d" % i, [128, 129], F32) for i in range(2)]
    ptr = [k.ps("d_ptr%d" % i, [128, 4, 128], BF16) for i in range(2)]
    cn = {"R": 0, "S": 0, "P": 0}

    def indexer(qi, t):
        qt = (2 * qi + 1) if parity else qi
        L = (qt + 1) * 128
        dq_, iq_, iw_, aw_, sg_, sc_ = dq[t], iq[t], iw[t], absw[t], sgn[t], sc[t]
        k.dma("sp", dq_[:], dqT_d[:, qi * 128:(qi + 1) * 128].rearrange("(h d) t -> d h t", h=4), writes=[dq_])
        k.dma("sp", iq_[:], iqT_d[:, qi * 128:(qi + 1) * 128].rearrange("(h e) t -> e h t", h=8), writes=[iq_])
        k.dma("sp", iw_[:], iw_d[qi * 128:(qi + 1) * 128, :], writes=[iw_])
        k.op("act", lambda e: e.activation(out=aw_[:], in_=iw_[:], func=AF.Abs, scale=WSC), reads=[iw_], writes=[aw_])
        k.op("act", lambda e: e.activation(out=sg_[:], in_=iw_[:], func=AF.Sign), reads=[iw_], writes=[sg_])
        for sb_ in range((L + 511) // 512):
            w = min(512, L - sb_ * 512)
            for h in range(8):
                pd = pdot[cn["R"] % 2]
                R_ = R[cn["R"] % 3]
                cn["R"] += 1
                k.op("pe", lambda e, pd=pd, h=h, sb_=sb_, w=w: e.matmul(pd[:, 0:w], lhsT=iq_[:, h, :], rhs=ikT[:, sb_ * 512: sb_ * 512 + w], start=True, stop=True),
                     reads=[iq_, ikT], writes=[pd])
                k.op("act", lambda e, pd=pd, R_=R_, h=h, w=w: e.activation(out=R_[:, 0:w], in_=pd[:, 0:w], func=AF.Relu, scale=aw_[:, h:h + 1]),
                     reads=[pd, aw_], writes=[R_])
                scs = sc_[:, sb_ * 512: sb_ * 512 + w]
                if h == 0:
                    k.op("dve", lambda e, R_=R_, scs=scs, w=w, h=h: e.tensor_scalar(scs, R_[:, 0:w], sg_[:, h:h + 1], None, op0=ALU.mult), reads=[R_, sg_], writes=[sc_])
                else:
                    k.op("dve", lambda e, R_=R_, scs=scs, w=w, h=h: e.scalar_tensor_tensor(out=scs, in0=R_[:, 0:w], scalar=sg_[:, h:h + 1], in1=scs, op0=ALU.mult, op1=ALU.add),
                         reads=[R_, sg_, sc_], writes=[sc_])
        k.op("pool", lambda e: e.tensor_tensor(out=sc_[:, L - CW:L], in0=sc_[:, L - CW:L], in1=caus[:], op=ALU.add), reads=[sc_, caus], writes=[sc_])

    def topk_init(qi, t):
        qt = (2 * qi + 1) if parity else qi
        L = (qt + 1) * 128
        v, sc_ = sv[t], sc[t]
        k.op("dve", lambda e: e.tensor_reduce(out=v["lo"][:], in_=sc_[:, 0:L - CW], axis=AX.X, op=ALU.min), reads=[sc_], writes=[v["lo"]])
        k.op("dve", lambda e: e.tensor_reduce(out=v["hi"][:], in_=sc_[:, 0:L], axis=AX.X, op=ALU.max), reads=[sc_], writes=[v["hi"]])
        k.op("dve", lambda e: e.tensor_tensor(out=v["w0"][:], in0=v["hi"][:], in1=v["lo"][:], op=ALU.subtract), reads=[v["hi"], v["lo"]], writes=[v["w0"]])

    def topk_iter(qi, t, itx):
        qt = (2 * qi + 1) if parity else qi
        L = (qt + 1) * 128
        v, sc_, j1, j2 = sv[t], sc[t], jk1[t], jk2[t]
        lo, w0, t1, nt2, cnt2, asum = v["lo"], v["w0"], v["t1"], v["nt2"], v["cnt2"], v["asum"]
        cf = 3.0 ** -(itx + 1)
        k.op("dve", lambda e: e.tensor_scalar(t1[:], w0[:], cf, lo[:, 0:1], op0=ALU.mult, op1=ALU.add), reads=[w0, lo], writes=[t1])
        k.op("dve", lambda e: e.tensor_scalar(nt2[:], w0[:], -2.0 * cf, lo[:, 0:1], op0=ALU.mult, op1=ALU.subtract), reads=[w0, lo], writes=[nt2])
        k.op("dve", lambda e: e.tensor_scalar(j1[:, 0:L], sc_[:, 0:L], t1[:, 0:1], 0.0, op0=ALU.is_ge, op1=ALU.add, accum_out=cnt2[:, 0:1]),
             reads=[sc_, t1, cnt2], writes=[j1, cnt2])
        k.op("act", lambda e: e.activation(out=j2[:, 0:L], in_=sc_[:, 0:L], func=AF.Sign, bias=nt2[:, 0:1], accum_out=cnt2[:, 1:2]),
             reads=[sc_, nt2, cnt2], writes=[j2, cnt2])
        k.op("dve", lambda e: e.tensor_scalar(asum[:], cnt2[:, 0:1], 255.5, None, op0=ALU.is_ge), reads=[cnt2], writes=[asum])
        k.op("dve", lambda e: e.scalar_tensor_tensor(out=asum[:], in0=cnt2[:, 1:2], scalar=511.5 - L, in1=asum[:], op0=ALU.is_ge, op1=ALU.add),
             reads=[cnt2, asum], writes=[asum])
        k.op("dve", lambda e: e.tensor_tensor(out=asum[:], in0=asum[:], in1=w0[:], op=ALU.mult), reads=[asum, w0], writes=[asum])
        k.op("dve", lambda e: e.scalar_tensor_tensor(out=lo[:], in0=asum[:], scalar=cf, in1=lo[:], op0=ALU.mult, op1=ALU.add), reads=[asum, lo], writes=[lo])

    def attend(qi, t):
        qt = (2 * qi + 1) if parity else qi
        L = (qt + 1) * 128
        sc_, dq_, v = sc[t], dq[t], sv[t]
        if qt >= 2:
            k.op("dve", lambda e: e.tensor_scalar(mask[:, 0:L], sc_[:, 0:L], v["lo"][:, 0:1], None, op0=ALU.is_ge), reads=[sc_, v["lo"]], writes=[mask])
        else:
            k.op("dve", lambda e: e.tensor_scalar(mask[:, 0:L], sc_[:, 0:L], -1e29, None, op0=ALU.is_ge), reads=[sc_], writes=[mask])
        for k0 in range(0, qt + 1, 4):
            nk = min(4, qt + 1 - k0)
            p_ = ptr[cn["P"] % 2]
            cn["P"] += 1
            for j in range(nk):
                k.op("pe", lambda e, p_=p_, j=j, k0=k0: e.transpose(p_[:, j, :], mask[:, (k0 + j) * 128:(k0 + j + 1) * 128], ident[:]), reads=[mask, ident], writes=[p_])
            k.op("act", lambda e, p_=p_, k0=k0, nk=nk: e.copy(maskT[:, k0:k0 + nk, :], p_[:, 0:nk, :]), reads=[p_], writes=[maskT])
        o_ = osb[qi % 2]
        stages = []
        for h in range(4):
            po_ = po[h % 2]
            groups = []
            nfar = max(qt - near + 1, 0)
            for k0 in range(0, nfar, 4):
                groups.append(("far", k0, min(4, nfar - k0)))
            for kt in range(nfar, qt + 1):
                groups.append(("near", kt, 1))
            for gi, (kind, k0, nk) in enumerate(groups):
                ps_ = pst[cn["S"] % 2]
                E_ = E[cn["S"] % 3]
                PT_ = PT[cn["S"] % 4]
                Lg_ = Lg[cn["S"] % 3]
                eng = "pool" if cn["S"] % 2 == 0 else "dve"
                cn["S"] += 1
                lastg = (gi == len(groups) - 1)

                def st0(kind=kind, k0=k0, nk=nk, ps_=ps_, E_=E_, PT_=PT_, Lg_=Lg_, eng=eng, h=h):
                    for j in range(nk):
                        k.op("pe", lambda e, j=j: e.matmul(ps_[:, j, :], lhsT=dkT[:, (k0 + j) * 128:(k0 + j + 1) * 128], rhs=dq_[:, h, :], start=True, stop=True),
                             reads=[dkT, dq_], writes=[ps_])
                    if kind == "far":
                        k.op("act", lambda e: e.activation(out=E_[:, 0:nk, :], in_=ps_[:, 0:nk, :], func=AF.Exp, scale=SCALE, bias=c31[:, h:h + 1]),
                             reads=[ps_, c31], writes=[E_])
                    else:
                        bi = k0 - (qt - near + 1)
                        k.op("dve", lambda e: e.scalar_tensor_tensor(out=Lg_[:], in0=ps_[:, 0, :], scalar=SCALE, in1=bias[:, h, bi, :], op0=ALU.mult, op1=ALU.add),
                             reads=[ps_, bias], writes=[Lg_])
                        k.op("act", lambda e: e.activation(out=E_[:, 0, :], in_=Lg_[:], func=AF.Exp), reads=[Lg_], writes=[E_])

                def st0b(k0=k0, nk=nk, E_=E_, PT_=PT_, eng=eng):
                    k.op(eng, lambda e: e.tensor_tensor(out=PT_[:, 0:nk, :], in0=E_[:, 0:nk, :], in1=maskT[:, k0:k0 + nk, :], op=ALU.mult),
                         reads=[E_, maskT], writes=[PT_])

                def st1(k0=k0, nk=nk, PT_=PT_, po_=po_, h=h, lastg=lastg):
                    for j in range(nk):
                        k.op("pe", lambda e, j=j: e.matmul(po_[:, :], lhsT=PT_[:, j, :], rhs=V1[:, k0 + j, :], start=(k0 + j == 0), stop=(k0 + j == qt)),
                             reads=[PT_, V1], writes=[po_])
                    if lastg:
                        k.op("dve", lambda e: e.reciprocal(rz[:], po_[:, 128:129]), reads=[po_], writes=[rz])
                        k.op("dve", lambda e: e.tensor_scalar(o_[:, h, :], po_[:, 0:128], rz[:, 0:1], None, op0=ALU.mult), reads=[po_, rz], writes=[o_])

                stages.append([st0, st0b, None, st1])
        run_pipeline(stages)
        p_ = ptr[cn["P"] % 2]
        cn["P"] += 1
        oT_ = oTs[qi % 2]
        for h in range(4):
            k.op("pe", lambda e, p_=p_, h=h: e.transpose(p_[:, h, :], o_[:, h, :], ident[:]), reads=[o_, ident], writes=[p_])
        k.op("act", lambda e: e.copy(oT_[:], p_[:]), reads=[p_], writes=[oT_])
        k.dma("pool", ocT_d[:, qi * 128:(qi + 1) * 128].rearrange("(h d) t -> d h t", h=4), oT_[:], reads=[oT_])

    for q0 in range(0, nqt, 2):
        pair = [(q0 + t, t) for t in range(2) if q0 + t < nqt]
        for (qi, t) in pair:
            indexer(qi, t)
        need = [(qi, t) for (qi, t) in pair if ((2 * qi + 1) if parity else qi) >= 2]
        for (qi, t) in need:
            topk_init(qi, t)
        for itx in range(13):
            for (qi, t) in need:
                topk_iter(qi, t, itx)
        for (qi, t) in pair:
            attend(qi, t)


def norm_rows(k, x_, h_, junk, ss, nfeat):
    k.op("act", lambda e: e.activation(out=junk[:], in_=x_[:], func=AF.Square, accum_out=ss[:]), reads=[x_], writes=[junk, ss])
    k.op("dve", lambda e: e.tensor_scalar(ss[:], ss[:], 1.0 / nfeat, EPS, op0=ALU.mult, op1=ALU.add), reads=[ss], writes=[ss])
    k.op("act", lambda e: e.activation(out=ss[:], in_=ss[:], func=AF.Sqrt), reads=[ss], writes=[ss])
    k.op("dve", lambda e: e.reciprocal(ss[:], ss[:]), reads=[ss], writes=[ss])
    k.op("dve", lambda e: e.tensor_scalar(h_[:], x_[:], ss[:, 0:1], None, op0=ALU.mult), reads=[x_, ss], writes=[h_])


def transpose_rows(k, h_, hT, gbuf, gi, ident, ptr, col0, nkc=16, ncols=128):
    for q in range(nkc // 4):
        p_ = ptr[q % 2]
        for j in range(4):
            kc = q * 4 + j
            k.op("pe", lambda e, p_=p_, kc=kc, j=j: e.transpose(p_[:, j, :], h_[:, kc * 128:(kc + 1) * 128], ident[:]), reads=[h_, ident], writes=[p_])
        for j in range(4):
            kc = q * 4 + j
            dst = hT[:, kc, col0:col0 + ncols]
            src = p_[:, j, 128 - ncols:128]
            if j % 2 == 0:
                k.op("act", lambda e, dst=dst, src=src, kc=kc: e.activation(out=dst, in_=src, func=AF.Copy, scale=gbuf[:, gi, kc:kc + 1]), reads=[p_, gbuf], writes=[hT])
            else:
                k.op("dve", lambda e, dst=dst, src=src, kc=kc: e.tensor_scalar(dst, src, gbuf[:, gi, kc:kc + 1], None, op0=ALU.mult), reads=[p_, gbuf], writes=[hT])


def p3a_body(k, x, sgT, oaT, obT, ocT, w_a, w_b, w_c, w_out, x1):
    P = [k.ps("ps%d" % i, [128, 512], F32) for i in range(8)]
    yT = k.sb("yT", [128, 16, T], BF16)
    st = ExitStack()
    oT = k.sb("oT", [128, 16, T], BF16, stack=st)
    k.dma("sp", oT[:, 0:8, :], oaT.rearrange("(kc p) t -> p kc t", p=128), writes=[oT])
    k.dma("sp", oT[:, 8:12, :], obT.rearrange("(kc p) t -> p kc t", p=128), writes=[oT])
    k.dma("sp", oT[:, 12:16, :], ocT.rearrange("(kc p) t -> p kc t", p=128), writes=[oT])
    wi = [k.sb("wi%d" % i, [128, 16, 256], BF16, stack=st) for i in range(2)]
    sgb = [k.sb("sgb%d" % i, [128, 3, 512], F32, stack=st) for i in range(4)]
    t0 = [k.sb("t0_%d" % i, [128, 512], F32, stack=st) for i in range(2)]
    t1 = [k.sb("t1_%d" % i, [128, 512], F32, stack=st) for i in range(2)]
    it = 0
    for cg in range(8):
        wb = wi[cg % 2]
        wload(k, wb, [(w_a, 0, 8), (w_b, 0, 4), (w_c, 0, 4)], cg * 256, 256)
        for cc in range(2):
            c = cg * 2 + cc
            for tb in range(4):
                sg_ = sgb[it % 4]
                t0_, t1_ = t0[it % 2], t1[it % 2]
                pY = [P[(it % 2) * 3 + i] for i in range(3)]
                it += 1
                k.dma("sp", sg_[:], sgT[:, tb * 512:(tb + 1) * 512].rearrange("(i f) t -> f i t", i=3)[c * 128:(c + 1) * 128], writes=[sg_])
                for i, (k0, k1) in enumerate(((0, 8), (8, 12), (12, 16))):
                    for kc in range(k0, k1):
                        k.op("pe", lambda e, i=i, kc=kc, k0=k0, k1=k1, pY=pY, wb=wb, cc=cc, tb=tb: e.matmul(
                            pY[i][:, :], lhsT=wb[:, kc, cc * 128:(cc + 1) * 128], rhs=oT[:, kc, tb * 512:(tb + 1) * 512],
                            start=(kc == k0), stop=(kc == k1 - 1)), reads=[wb, oT], writes=[pY[i]])
                k.op("dve", lambda e, t0_=t0_, pY=pY, sg_=sg_: e.tensor_tensor(out=t0_[:], in0=pY[0][:], in1=sg_[:, 0, :], op=ALU.mult), reads=[pY[0], sg_], writes=[t0_])
                k.op("dve", lambda e, t1_=t1_, pY=pY, sg_=sg_: e.tensor_tensor(out=t1_[:], in0=pY[1][:], in1=sg_[:, 1, :], op=ALU.mult), reads=[pY[1], sg_], writes=[t1_])
                k.op("dve", lambda e, t0_=t0_, t1_=t1_: e.tensor_tensor(out=t0_[:], in0=t0_[:], in1=t1_[:], op=ALU.add), reads=[t0_, t1_], writes=[t0_])
                k.op("dve", lambda e, t1_=t1_, pY=pY, sg_=sg_: e.tensor_tensor(out=t1_[:], in0=pY[2][:], in1=sg_[:, 2, :], op=ALU.mult), reads=[pY[2], sg_], writes=[t1_])
                k.op("dve", lambda e, t0_=t0_, t1_=t1_, c=c, tb=tb: e.tensor_tensor(out=yT[:, c, tb * 512:(tb + 1) * 512], in0=t0_[:], in1=t1_[:], op=ALU.add),
                     reads=[t0_, t1_], writes=[yT])
    k.barrier()
    st.close()
    wo = k.sb("wo", [128, 16, D], BF16)
    for cg in range(4):
        k.dma("pool", wo[:, :, cg * 512:(cg + 1) * 512], w_out[:, cg * 512:(cg + 1) * 512].rearrange("(kc p) c -> p kc c", p=128), writes=[wo])
    xt = [k.sb("xt%d" % i, [128, D], F32) for i in range(2)]
    for t in range(T // 128):
        x_ = xt[t % 2]
        k.dma("sp", x_[:], x[t * 128:(t + 1) * 128, :], writes=[x_])
        for cb in range(4):
            p_ = P[(t * 4 + cb) % 8]
            for kc in range(16):
                k.op("pe", lambda e, p_=p_, kc=kc, t=t, cb=cb: e.matmul(p_[:, :], lhsT=yT[:, kc, t * 128:(t + 1) * 128], rhs=wo[:, kc, cb * 512:(cb + 1) * 512],
                                                                       start=(kc == 0), stop=(kc == 15)), reads=[yT, wo], writes=[p_])
            k.op("dve", lambda e, p_=p_, x_=x_, cb=cb: e.tensor_tensor(out=x_[:, cb * 512:(cb + 1) * 512], in0=x_[:, cb * 512:(cb + 1) * 512], in1=p_[:, :], op=ALU.add),
                 reads=[x_, p_], writes=[x_])
        k.dma("sp", x1[t * 128:(t + 1) * 128, :], x_[:], reads=[x_])


def p3b_body(k, c, x1, pT_d, gn, gbf_d, w_gate, w_up, w_down, cw_d, cb_d, wpg, wpp, x3, final):
    P = [k.ps("ps%d" % i, [128, 512], F32) for i in range(6)]
    ptr = [k.ps("ptr%d" % i, [128, 4, 128], BF16) for i in range(2)]
    ident = c["ident"]
    gnb = k.sb("gn_s", [128, 3, 16], F32)
    k.dma("sp", gnb[:], gn[:, :, :], writes=[gnb])
    cw = k.sb("cw_s", [128, NFC, 3], F32)
    cb = k.sb("cb_s", [128, NFC], F32)
    k.dma("sp", cw[:], cw_d[:, :, :], writes=[cw])
    k.dma("sp", cb[:], cb_d[:, :], writes=[cb])
    if final:
        gbf = k.sb("gbf_s", [128, D], F32)
        k.dma("sp", gbf[:], gbf_d[:, :], writes=[gbf])
    hTs = [k.sb("hT%d" % i, [128, 16, 514], BF16) for i in range(2)]

    mT = k.sb("mT", [128, NFC, 512], BF16)
    wg = [k.sb("wg%d" % i, [128, 16, 128], BF16) for i in range(2)]
    wu = [k.sb("wu%d" % i, [128, 16, 128], BF16) for i in range(2)]
    wd = [k.sb("wd%d" % i, [128, 22, 512], BF16) for i in range(2)]
    x2 = k.sb("x2", [128, 4, D], F32)
    xt = k.sb("xt", [128, D], F32)
    hb = k.sb("hb", [128, D], BF16)
    junk = hb
    ss = k.sb("ss", [128, 1], F32)
    A_sb = [k.sb("A_sb%d" % i, [128, 514], F32) for i in range(2)]
    cv = [k.sb("cv%d" % i, [128, 512], F32) for i in range(2)]
    gg = [k.sb("gg%d" % i, [128, 512], F32) for i in range(2)]
    sgG = gg
    pTb = k.sb("pTb", [128, 2, 512], BF16)
    def prep(tb):
        hT = hTs[tb % 2]
        if tb == 0:
            k.op("dve", lambda e: e.memset(hT[:, :, 0:2], 0.0), writes=[hT])
        else:
            k.dma("sp", xt[:], x1[tb * 512 - 128: tb * 512, :], writes=[xt])
            norm_rows(k, xt, hb, junk, ss, D)
            transpose_rows(k, hb, hT, gnb, 0, ident, ptr, 0, ncols=2)
        for j in range(4):
            k.dma("sp", xt[:], x1[tb * 512 + j * 128: tb * 512 + (j + 1) * 128, :], writes=[xt])
            norm_rows(k, xt, hb, junk, ss, D)
            transpose_rows(k, hb, hT, gnb, 0, ident, ptr, 2 + j * 128)

    it = 0

    def block(tb):
        nonlocal it
        if tb == 0:
            prep(0)
        hT = hTs[tb % 2]
        for j in range(4):
            k.dma("sp", x2[:, j, :], x1[tb * 512 + j * 128: tb * 512 + (j + 1) * 128, :], writes=[x2])
        for fc in range(NFC):
            if fc == NFC // 2 and tb + 1 < S // 512:
                prep(tb + 1)
            wg_, wu_ = wg[fc % 2], wu[fc % 2]
            wload(k, wg_, [(w_gate, 0, 16)], fc * 128, 128)
            wload(k, wu_, [(w_up, 0, 16)], fc * 128, 128)
            pa, pu, ph = P[(it % 2) * 3], P[(it % 2) * 3 + 1], P[(it % 2) * 3 + 2]
            A_, cv_, gg_ = A_sb[it % 2], cv[it % 2], gg[it % 2]
            it += 1
            for kc in range(16):
                k.op("pe", lambda e, pa=pa, kc=kc, wg_=wg_: e.matmul(pa[:, :], lhsT=wg_[:, kc, :], rhs=hT[:, kc, 2:514], start=(kc == 0), stop=(kc == 15)),
                     reads=[wg_, hT], writes=[pa])
            for kc in range(16):
                k.op("pe", lambda e, ph=ph, kc=kc, wg_=wg_: e.matmul(ph[:, 0:2], lhsT=wg_[:, kc, :], rhs=hT[:, kc, 0:2], start=(kc == 0), stop=(kc == 15)),
                     reads=[wg_, hT], writes=[ph])
            for kc in range(16):
                k.op("pe", lambda e, pu=pu, kc=kc, wu_=wu_: e.matmul(pu[:, :], lhsT=wu_[:, kc, :], rhs=hT[:, kc, 2:514], start=(kc == 0), stop=(kc == 15)),
                     reads=[wu_, hT], writes=[pu])
            k.op("act", lambda e, A_=A_, pa=pa: e.copy(A_[:, 2:514], pa[:, :]), reads=[pa], writes=[A_])
            k.op("act", lambda e, A_=A_, ph=ph: e.copy(A_[:, 0:2], ph[:, 0:2]), reads=[ph], writes=[A_])
            k.op("dve", lambda e, A_=A_, cv_=cv_, fc=fc: e.tensor_scalar(cv_[:], A_[:, 2:514], cw[:, fc, 2:3], cb[:, fc:fc + 1], op0=ALU.mult, op1=ALU.add),
                 reads=[A_, cw, cb], writes=[cv_])
            k.op("dve", lambda e, A_=A_, cv_=cv_, fc=fc: e.scalar_tensor_tensor(out=cv_[:], in0=A_[:, 1:513], scalar=cw[:, fc, 1:2], in1=cv_[:], op0=ALU.mult, op1=ALU.add),
                 reads=[A_, cw, cv_], writes=[cv_])
            k.op("dve", lambda e, A_=A_, cv_=cv_, fc=fc: e.scalar_tensor_tensor(out=cv_[:], in0=A_[:, 0:512], scalar=cw[:, fc, 0:1], in1=cv_[:], op0=ALU.mult, op1=ALU.add),
                 reads=[A_, cw, cv_], writes=[cv_])
            k.op("act", lambda e, cv_=cv_, gg_=gg_: e.activation(out=gg_[:], in_=cv_[:], func=AF.Gelu_apprx_tanh), reads=[cv_], writes=[gg_])
            k.op("dve", lambda e, gg_=gg_, pu=pu, fc=fc: e.tensor_tensor(out=mT[:, fc, :], in0=gg_[:], in1=pu[:, :], op=ALU.mult), reads=[gg_, pu], writes=[mT])
        for cbk in range(4):
            pd = [P[j] for j in range(4)]
            for piece in range(2):
                wd_ = wd[(cbk * 2 + piece) % 2]
                wload(k, wd_, [(w_down, piece * 22 * 128, 22)], cbk * 512, 512)
                for j in range(4):
                    for f in range(22):
                        fc = piece * 22 + f
                        k.op("pe", lambda e, j=j, f=f, fc=fc, wd_=wd_, pd=pd: e.matmul(pd[j][:, :], lhsT=mT[:, fc, j * 128:(j + 1) * 128], rhs=wd_[:, f, :],
                                                                                  start=(fc == 0), stop=(fc == NFC - 1)), reads=[mT, wd_], writes=[pd[j]])
            for j in range(4):
                k.op("dve", lambda e, j=j, cbk=cbk, pd=pd: e.tensor_tensor(out=x2[:, j, cbk * 512:(cbk + 1) * 512], in0=x2[:, j, cbk * 512:(cbk + 1) * 512], in1=pd[j][:, :], op=ALU.add),
                     reads=[x2, pd[j]], writes=[x2])
        for j in range(4):
            k.op("act", lambda e, j=j: e.copy(xt[:], x2[:, j, :]), reads=[x2], writes=[xt])
            norm_rows(k, xt, hb, junk, ss, D)
            transpose_rows(k, hb, hT, gnb, 1, ident, ptr, 2 + j * 128)
        k.dma("pool", pTb[:], pT_d[:, tb * 512:(tb + 1) * 512].rearrange("(kc p) t -> p kc t", p=128), writes=[pTb])
        for cbk in range(4):
            wd_ = wd[cbk % 2]
            wload(k, wd_, [(wpg, 0, 16), (wpp, 0, 2)], cbk * 512, 512)
            for j in range(4):
                pG, pP = P[4 + (j % 2)], P[j % 2]
                sg_ = sgG[j % 2]
                for kc in range(16):
                    k.op("pe", lambda e, pG=pG, kc=kc, j=j, wd_=wd_: e.matmul(pG[:, :], lhsT=hT[:, kc, 2 + j * 128: 2 + (j + 1) * 128], rhs=wd_[:, kc, :],
                                                                           start=(kc == 0), stop=(kc == 15)), reads=[hT, wd_], writes=[pG])
                for kc in range(2):
                    k.op("pe", lambda e, pP=pP, kc=kc, j=j, wd_=wd_: e.matmul(pP[:, :], lhsT=pTb[:, kc, j * 128:(j + 1) * 128], rhs=wd_[:, 16 + kc, :],
                                                                           start=(kc == 0), stop=(kc == 1)), reads=[pTb, wd_], writes=[pP])
                k.op("act", lambda e, pG=pG, sg_=sg_: e.activation(out=sg_[:], in_=pG[:, :], func=AF.Sigmoid), reads=[pG], writes=[sg_])
                k.op("dve", lambda e, pP=pP, sg_=sg_: e.tensor_tensor(out=sg_[:], in0=sg_[:], in1=pP[:, :], op=ALU.mult), reads=[pP, sg_], writes=[sg_])
                k.op("dve", lambda e, sg_=sg_, j=j, cbk=cbk: e.tensor_tensor(out=x2[:, j, cbk * 512:(cbk + 1) * 512], in0=x2[:, j, cbk * 512:(cbk + 1) * 512], in1=sg_[:], op=ALU.add),
                     reads=[x2, sg_], writes=[x2])
        for j in range(4):
            if final:
                k.op("act", lambda e, j=j: e.activation(out=junk[:], in_=x2[:, j, :], func=AF.Square, accum_out=ss[:]), reads=[x2], writes=[junk, ss])
                k.op("dve", lambda e: e.tensor_scalar(ss[:], ss[:], 1.0 / D, EPS, op0=ALU.mult, op1=ALU.add), reads=[ss], writes=[ss])
                k.op("act", lambda e: e.activation(out=ss[:], in_=ss[:], func=AF.Sqrt), reads=[ss], writes=[ss])
                k.op("dve", lambda e: e.reciprocal(ss[:], ss[:]), reads=[ss], writes=[ss])
                k.op("dve", lambda e, j=j: e.scalar_tensor_tensor(out=x2[:, j, :], in0=x2[:, j, :], scalar=ss[:, 0:1], in1=gbf[:], op0=ALU.mult, op1=ALU.mult),
                     reads=[x2, ss, gbf], writes=[x2])
            k.dma("sp", x3[tb * 512 + j * 128: tb * 512 + (j + 1) * 128, :], x2[:, j, :], reads=[x2])

    for tb_ in range(S // 512):
        block(tb_)


W_SHAPES = [
    ("w_in", [D, INC]), ("gb", [128, D]), ("kvg", [128, 256]), ("wuk", [256, 128]), ("wuv", [256, 128]),
    ("ck_w1", [4096, 256]), ("ck_w2", [256, 128]), ("ck_peT", [128, 32]),
    ("cv_w1", [4096, 256]), ("cv_w2", [256, 128]), ("cv_peT", [128, 32]),
    ("w_a", [1024, D]), ("w_b", [512, D]), ("w_c", [512, D]), ("w_out", [D, D]),
    ("gn", [128, 3, 16]), ("w_gate", [D, DFF]), ("w_up", [D, DFF]), ("w_down", [DFF, D]),
    ("cw", [128, NFC, 3]), ("cb", [128, NFC]), ("wpg", [D, D]), ("wpp", [256, D]),
]
C_SHAPES = [
    ("wslc", [2, 128, 4, 1024], F32), ("wwin", [2, 128, 4, 1408], F32), ("mext", [2, 128, 4, 503], F32), ("nc31", [2, 128, 4], F32),
    ("keep", [128, 128], F32), ("add", [128, 128], F32), ("ov", [128, 2, 64], BF16), ("eexp", [64, S], BF16),
    ("dbias", [128, 4, 2, 128], F32), ("dc31", [128, 4], F32), ("dcaus", [128, 128], F32), ("gbf", [128, D], F32),
]


def build_prog(nl, final):
    k = KB()
    c = consts_common(k)
    x_in = k.dram_in("x", [S, D], F32)
    pT = k.dram_in("pT", [nl, 256, S], F32)
    W = {nm: k.dram_in(nm, [nl] + shp, F32) for nm, shp in W_SHAPES}
    C = {nm: k.dram_in(nm, shp, dt) for nm, shp, dt in C_SHAPES}
    out = k.dram_out("out", [S, D], F32)
    sc = {}
    for (name, c0, n, mode, dt, act) in SEGS:
        if mode == "FM":
            sc[name] = k.dram_tmp("t_" + name, [n, S], dt)
        elif mode == "TM":
            sc[name] = k.dram_tmp("t_" + name, [S, n], dt)
    sc["dkT"] = k.dram_tmp("t_dkT", [128, S], BF16)
    sc["dv"] = k.dram_tmp("t_dv", [S, 128], BF16)
    sc["oaT"] = k.dram_tmp("t_oaT", [1024, S], BF16)
    sc["obT"] = k.dram_tmp("t_obT", [512, S], BF16)
    sc["ocT"] = k.dram_tmp("t_ocT", [512, S], BF16)
    X1 = k.dram_tmp("t_X1", [S, D], F32)
    XS = [k.dram_tmp("t_XS%d" % i, [S, D], F32) for i in range(2)]
    for li in range(nl):
        xin = x_in if li == 0 else XS[(li - 1) % 2]
        xout = out if li == nl - 1 else XS[li % 2]
        for half in range(2):
            k.begin()
            p1_body(k, c, xin, W["w_in"][li], W["gb"][li], W["kvg"][li], W["wuk"][li], W["wuv"][li], sc, half * T)
            k.end()
        for g in range(2):
            k.begin()
            d = {"qnT": sc["qnT"][g * 512:(g + 1) * 512, :], "ng": sc["ng"][:, g * 12:(g + 1) * 12]}
            for nm in ("kcT", "vcT", "ksT", "kwT"):
                d[nm] = sc[nm][g * 128:(g + 1) * 128, :]
            for nm in ("vs", "vw"):
                d[nm] = sc[nm][:, g * 128:(g + 1) * 128]
            for nm in ("ck_w1", "ck_w2", "ck_peT", "cv_w1", "cv_w2", "cv_peT"):
                d[nm] = W[nm][li]
            for nm in ("wslc", "wwin", "mext", "nc31"):
                d[nm] = C[nm][g]
            for nm in ("keep", "add", "ov", "eexp"):
                d[nm] = C[nm]
            build_nsa(k, c, d, sc["oaT"][g * 512:(g + 1) * 512, :])
            k.end()
        k.begin()
        build_sb(k, c, sc["sbqT"], sc["sbkT"], sc["sbv"], sc["obT"], 4)
        k.end()
        k.begin()
        build_dsa(k, c, sc["dqT"], sc["dkT"], sc["dv"], sc["iqT"], sc["ikT"], sc["iw"], C["dbias"], C["dc31"], C["dcaus"], sc["ocT"], nqt=32, near=2, parity=False)
        k.end()
        for half in range(2):
            ts = slice(half * T, (half + 1) * T)
            k.begin()
            p3a_body(k, xin[ts, :], sc["sgT"][:, ts], sc["oaT"][:, ts], sc["obT"][:, ts], sc["ocT"][:, ts],
                     W["w_a"][li], W["w_b"][li], W["w_c"][li], W["w_out"][li], X1[ts, :])
            k.end()
        k.begin()
        p3b_body(k, c, X1, pT[li], W["gn"][li], C["gbf"], W["w_gate"][li], W["w_up"][li], W["w_down"][li], W["cw"][li], W["cb"][li],
                 W["wpg"][li], W["wpp"][li], xout, final and li == nl - 1)
        k.end()
    return k.finish()


def _bc(v, rows=128):
    return np.ascontiguousarray(np.broadcast_to(np.asarray(v, np.float32)[None, :], (rows, v.shape[0])))


def _pp(v):
    return np.ascontiguousarray(np.asarray(v, np.float32).reshape(-1, 128).T)


def host_layout(inp, layers):
    Ls = list(layers)
    g = lambda nm: np.asarray(inp[nm], np.float32)
    Wd = {
        "w_in": g("w_in")[Ls], "wuk": g("dsa_w_uk")[Ls], "wuv": g("dsa_w_uv")[Ls],
        "ck_w1": g("cmp_k_w1")[Ls], "ck_w2": g("cmp_k_w2")[Ls], "cv_w1": g("cmp_v_w1")[Ls], "cv_w2": g("cmp_v_w2")[Ls],
        "w_a": g("w_proj_a")[Ls], "w_b": g("w_proj_b")[Ls], "w_c": g("w_proj_c")[Ls], "w_out": g("w_out")[Ls],
        "w_gate": g("ffn_w_gate")[Ls], "w_up": g("ffn_w_up")[Ls], "w_down": g("ffn_w_down")[Ls], "wpg": g("ple_w_gate")[Ls], "wpp": g("ple_w_proj")[Ls],
    }
    Wd["gb"] = np.stack([_bc(g("norm_mix")[L]) for L in Ls])
    Wd["kvg"] = np.stack([_bc(g("dsa_kv_norm")[L]) for L in Ls])
    Wd["ck_peT"] = np.stack([np.ascontiguousarray(g("cmp_k_pe")[L].T) for L in Ls])
    Wd["cv_peT"] = np.stack([np.ascontiguousarray(g("cmp_v_pe")[L].T) for L in Ls])
    Wd["gn"] = np.stack([np.stack([_pp(g("norm_ffn")[L]), _pp(g("norm_ple")[L]), _pp(g("norm_final"))], 1) for L in Ls])
    Wd["cw"] = np.stack([np.ascontiguousarray(g("ffn_conv_w")[L].reshape(3, NFC, 128).transpose(2, 1, 0)) for L in Ls])
    Wd["cb"] = np.stack([np.ascontiguousarray(g("ffn_conv_b")[L].reshape(NFC, 128).T) for L in Ls])
    tab = g("rel_bias_table")
    n0, n1 = nsa_consts(tab, 0), nsa_consts(tab, 1)
    Cd = {nm: np.stack([n0[nm], n1[nm]]) for nm in ("wslc", "wwin", "mext", "nc31")}
    Cd["keep"], Cd["add"] = n0["keep"], n0["add"]
    Cd["ov"], Cd["eexp"] = n0["ov"].astype(NPBF), n0["eexp"].astype(NPBF)
    Cd["dbias"], Cd["dc31"], Cd["dcaus"] = dsa_consts_exact(tab)
    Cd["gbf"] = _bc(g("norm_final"))
    return Wd, Cd


_PROGS = {}


def _prog(nl, final):
    key = (nl, final)
    if key not in _PROGS:
        _PROGS[key] = build_prog(nl, final)
    return _PROGS[key]


LAYERS_PER_LAUNCH = 4


def kernel(**inp):
    x = np.asarray(inp["x"], np.float32)
    p = np.asarray(inp["p"], np.float32)
    B = x.shape[0]
    depth = p.shape[0]
    cur = [np.ascontiguousarray(x[b]) for b in range(B)]
    for l0 in range(0, depth, LAYERS_PER_LAUNCH):
        Ls = list(range(l0, min(depth, l0 + LAYERS_PER_LAUNCH)))
        final = (Ls[-1] == depth - 1)
        Wd, Cd = host_layout(inp, Ls)
        nc = _prog(len(Ls), final)
        in_maps = []
        for b in range(B):
            m = {"x": cur[b], "pT": np.ascontiguousarray(np.stack([p[L, b].T for L in Ls]))}
            m.update(Wd)
            m.update(Cd)
            in_maps.append(m)
        res = run_bass_kernel_spmd(nc, in_maps, core_ids=list(range(B)))
        cur = [np.asarray(res.results[b]["out"], np.float32) for b in range(B)]
    return np.stack(cur).astype(np.float32)
```

```python
import math
import numpy as np
from contextlib import ExitStack
import ml_dtypes
import concourse.bass as bass
import concourse.mybir as mybir
from concourse.bass_utils import run_bass_kernel_spmd

F32 = mybir.dt.float32
BF16 = mybir.dt.bfloat16
AF = mybir.ActivationFunctionType
ALU = mybir.AluOpType
AX = mybir.AxisListType
NPBF = ml_dtypes.bfloat16

SAME_ENGINE_SYNC = True
S = 4096
D = 2048
T = 2048
INC = 11616
DFF = 5632
NFC = DFF // 128
EPS = 1e-6
SCALE = 128 ** -0.5
NEG = -30000.0


class Buf:
    __slots__ = ("t", "w", "r", "dsem", "dcount", "name")

    def __init__(self, t, name):
        self.t = t
        self.name = name
        self.w = {}
        self.r = {}
        self.dsem = None
        self.dcount = 0

    def __getitem__(self, idx):
        return self.t[idx]


class KB:
    ENG = ("pe", "act", "dve", "pool", "sp")

    def __init__(self):
        self.nc = bass.Bass("TRN2", target_bir_lowering=False)
        self.es = ExitStack()
        self.ops = {e: [] for e in self.ENG}
        self.cnt = {e: 0 for e in self.ENG}
        self.seen = {e: {} for e in self.ENG}
        self.sems = {}
        self.dma_latest = {}
        self.free_sems = []
        self.phase_owned = []
        self.phase = None
        self.nsem = 0
        self.uid = 0
        for e in ("pe", "act", "dve", "pool"):
            self.sems[e] = self.es.enter_context(self.nc.semaphore("s_" + e))

    def dram_in(self, name, shape, dt):
        return self.nc.dram_tensor(name, list(shape), dt, kind="ExternalInput").ap()

    def dram_out(self, name, shape, dt):
        return self.nc.dram_tensor(name, list(shape), dt, kind="ExternalOutput").ap()

    def dram_tmp(self, name, shape, dt):
        return self.nc.dram_tensor(name, list(shape), dt).ap()

    def _nm(self, name):
        self.uid += 1
        return "%s_%d" % (name, self.uid)

    def sb(self, name, shape, dt, stack=None, persist=False):
        st = stack or (self.es if (persist or self.phase is None) else self.phase)
        nm = self._nm(name)
        t = st.enter_context(self.nc.sbuf_tensor(nm, list(shape), dt))
        return Buf(t, nm)

    def ps(self, name, shape, dt=F32, stack=None, persist=False):
        st = stack or (self.es if (persist or self.phase is None) else self.phase)
        nm = self._nm(name)
        t = st.enter_context(self.nc.psum_tensor(nm, list(shape), dt))
        return Buf(t, nm)

    def begin(self):
        assert self.phase is None
        self.phase = ExitStack()
        self.phase_owned = []

    def end(self):
        self.barrier(full=True)
        for b in self.phase_owned:
            self.free_sems.append((b.dsem, b.dcount))
            b.dsem = None
        self.phase_owned = []
        self.phase.close()
        self.phase = None

    def _deps(self, reads, writes):
        d = {}
        raw = {}
        for b in reads:
            for k, v in b.w.items():
                if d.get(k, 0) < v:
                    d[k] = v
                if raw.get(k, 0) < v:
                    raw[k] = v
        for b in writes:
            for k, v in b.w.items():
                if d.get(k, 0) < v:
                    d[k] = v
            for k, v in b.r.items():
                if d.get(k, 0) < v:
                    d[k] = v
        self._raw = raw
        return d

    def _emit_waits(self, e, deps):
        seen = self.seen[e]
        for k, v in deps.items():
            if k == e:
                if not SAME_ENGINE_SYNC or e == "pe":
                    continue
                v = self._raw.get(k, 0)
                if v == 0:
                    continue
            if seen.get(k, 0) >= v:
                continue
            seen[k] = v
            sem = self.sems[k]
            self.ops[e].append(lambda eng, s=sem, vv=v: eng.wait_ge(s, vv))

    def _commit(self, tok_key, tok_val, reads, writes):
        for b in reads:
            if b.r.get(tok_key, 0) < tok_val:
                b.r[tok_key] = tok_val
        for b in writes:
            b.w[tok_key] = tok_val
            b.r = {}

    def op(self, e, fn, reads=(), writes=()):
        deps = self._deps(reads, writes)
        self._emit_waits(e, deps)
        self.cnt[e] += 1
        sem = self.sems[e]
        self.ops[e].append(lambda eng, f=fn, s=sem: f(eng).then_inc(s, 1))
        self._commit(e, self.cnt[e], reads, writes)

    def dma(self, q, out_ap, in_ap, reads=(), writes=(), **kw):
        owner = writes[0] if len(writes) else reads[0]
        if owner.dsem is None:
            if self.free_sems:
                key, base = self.free_sems.pop()
            else:
                key = "d%d" % self.nsem
                self.nsem += 1
                self.sems[key] = self.es.enter_context(self.nc.semaphore("s_" + key))
                base = 0
            owner.dsem = key
            owner.dcount = base
            self.phase_owned.append(owner)
        deps = self._deps(reads, writes)
        self._emit_waits(q, deps)
        owner.dcount += 16
        sem = self.sems[owner.dsem]
        self.ops[q].append(lambda eng, o=out_ap, i=in_ap, s=sem, k=kw: eng.dma_start(out=o, in_=i, **k).then_inc(s, 16))
        self._commit(owner.dsem, owner.dcount, reads, writes)
        self.dma_latest[owner.dsem] = owner.dcount

    def barrier(self, full=False):
        allv = {e: self.cnt[e] for e in ("pe", "act", "dve", "pool")}
        if full:
            allv.update(self.dma_latest)
        for e in self.ENG:
            self._emit_waits(e, dict(allv))

    def finish(self):
        self.barrier(full=True)
        nc = self.nc
        ops = self.ops
        with nc.Block() as block:
            @block.tensor
            def _(eng):
                for f in ops["pe"]:
                    f(eng)

            @block.scalar
            def _(eng):
                for f in ops["act"]:
                    f(eng)

            @block.vector
            def _(eng):
                for f in ops["dve"]:
                    f(eng)

            @block.gpsimd
            def _(eng):
                for f in ops["pool"]:
                    f(eng)

            @block.sync
            def _(eng):
                for f in ops["sp"]:
                    f(eng)
        self.es.close()
        return nc

    def stats(self):
        return {e: len(self.ops[e]) for e in self.ENG}


NEGB = -30000.0

def t5_bucket_np(dist):
    n = np.maximum(dist, 0)
    nf = np.maximum(n, 1).astype(np.float32)
    large = 16 + (np.log(nf / np.float32(16)) / np.float32(math.log(128 / 16)) * np.float32(16)).astype(np.int32)
    large = np.minimum(large, 31)
    return np.where(n < 16, n, large)

def near_tiles(tabcol):
    s = np.arange(128)[:, None]; t = np.arange(128)[None, :]
    d0 = t - s
    diag = np.where(d0 >= 0, tabcol[t5_bucket_np(d0)], np.float32(NEGB)).astype(np.float32)
    off1 = tabcol[t5_bucket_np(d0 + 128)].astype(np.float32)
    c31 = np.full((128, 128), tabcol[31], np.float32)
    negt = np.full((128, 128), NEGB, np.float32)
    return diag, off1, c31, negt

def dsa_consts(tab, g):
    db = np.zeros((128, 4, 3, 128), np.float32)
    for h in range(4):
        diag, off1, c31, negt = near_tiles(tab[:, 8 + h])
        tiles = (off1, diag, negt) if g == 0 else (c31, off1, diag)
        for i in range(3):
            db[:, h, i, :] = tiles[i]
    dc31 = np.broadcast_to(tab[31, 8:12][None, :], (128, 4)).astype(np.float32).copy()
    t = np.arange(128)[:, None]; s = np.arange(128)[None, :]
    tri = np.where(s <= t, 0.0, -1e30).astype(np.float32)
    if g == 0:
        caus = np.concatenate([tri, np.full((128, 128), -1e30, np.float32)], 1)
    else:
        caus = np.concatenate([np.zeros((128, 128), np.float32), tri], 1)
    return db, dc31, caus

def nsa_consts(tab, g):
    wslc = np.zeros((128, 4, 8, 128), np.float32)
    wwin = np.zeros((128, 4, 11, 128), np.float32)
    mext = np.zeros((128, 4, 503), np.float32)
    s = np.arange(128)[:, None]; t = np.arange(128)[None, :]
    for hh in range(4):
        col = tab[:, g * 4 + hh]
        diag, off1, c31, negt = near_tiles(col)
        wedge = np.where(t < s, col[31], np.float32(NEGB)).astype(np.float32)
        for i, tl in enumerate([negt, negt, negt, diag, off1, c31, c31, c31]):
            wslc[:, hh, i, :] = tl
        for i, tl in enumerate([negt, negt, negt, diag, off1, c31, c31, wedge, negt, negt, negt]):
            wwin[:, hh, i, :] = tl
        i_ = np.arange(128)[:, None]; m_ = np.arange(503)[None, :]
        dist = i_ - 16 * (m_ - 248) - 31
        mext[:, hh, :] = np.where(dist >= 0, col[t5_bucket_np(dist)], np.float32(NEGB))
    c31v = np.broadcast_to(tab[31, g * 4:g * 4 + 4][None, :], (128, 4)).astype(np.float32).copy()
    keep = np.zeros((128, 128), np.float32); add = np.zeros((128, 128), np.float32)
    for i in range(128):
        for m in range(128):
            r = m - 64
            if r >= 2: kp, ad = 0.0, -1.0
            elif r == 1: kp, ad = (0.0, 2e9) if i >= 64 else (0.0, -1.0)
            elif r == 0: kp, ad = 0.0, 1e9
            elif r == -1: kp, ad = (0.0, 2e9) if i < 64 else (1.0, 0.0)
            else: kp, ad = 1.0, 0.0
            keep[i, m] = kp; add[i, m] = ad
    c = np.arange(256)[:, None]; j = np.arange(64)[None, :]
    ov = ((16 * c < 64 * j + 64) & (16 * c + 32 > 64 * j) & (c < 255)).astype(np.float32)
    ov = ov.reshape(2, 128, 64).transpose(1, 0, 2).copy()
    jj = np.arange(64)[:, None]; ss = np.arange(4096)[None, :]
    eexp = (ss // 64 == jj).astype(np.float32)
    return dict(wslc=wslc.reshape(128, 4, 1024), wwin=wwin.reshape(128, 4, 1408), mext=mext, nc31=c31v, keep=keep, add=add, ov=ov, eexp=eexp)


def dsa_consts_exact(tab):
    db = np.zeros((128, 4, 2, 128), np.float32)
    for h in range(4):
        diag, off1, c31, negt = near_tiles(tab[:, 8 + h])
        db[:, h, 0, :] = off1
        db[:, h, 1, :] = diag
    dc31 = np.broadcast_to(tab[31, 8:12][None, :], (128, 4)).astype(np.float32).copy()
    t = np.arange(128)[:, None]; s = np.arange(128)[None, :]
    tri = np.where(s <= t, 0.0, -1e30).astype(np.float32)
    return db, dc31, tri


SEGS = [
    ("sgT", 0, 6144, "FM", F32, "sig"),
    ("qnT", 6144, 1024, "FM", BF16, None),
    ("kcT", 7168, 256, "FM", BF16, None),
    ("vcT", 7424, 256, "FM", BF16, None),
    ("ksT", 7680, 256, "FM", BF16, None),
    ("vs", 7936, 256, "TM", BF16, None),
    ("kwT", 8192, 256, "FM", BF16, None),
    ("vw", 8448, 256, "TM", BF16, None),
    ("ng", 8704, 24, "TM", F32, None),
    ("sbqT", 8728, 512, "FM", BF16, None),
    ("sbkT", 9240, 512, "FM", BF16, None),
    ("sbv", 9752, 512, "TM", BF16, None),
    ("dqT", 10264, 512, "FM", BF16, None),
    ("ckv", 10776, 256, "CKV", None, None),
    ("iqT", 11032, 512, "FM", BF16, None),
    ("ikT", 11544, 64, "FM", BF16, None),
    ("iw", 11608, 8, "TM", F32, None),
]


def make_identity(k, name="ident"):
    idf = k.sb(name + "_f", [128, 128], F32, persist=True)
    idn = k.sb(name, [128, 128], BF16, persist=True)
    k.op("pool", lambda e: e.memset(idf[:], 0.0), writes=[idf])
    k.op("pool", lambda e: e.affine_select(out=idf[:], in_=idf[:], pattern=[[-1, 128]], compare_op=ALU.not_equal,
                                           fill=1.0, base=0, channel_multiplier=1), reads=[idf], writes=[idf])
    k.op("dve", lambda e: e.tensor_copy(idn[:], idf[:]), reads=[idf], writes=[idn])
    return idn, idf


def rmsnorm_to_hT(k, x_ap, gb, hT, ident, ntiles, tcol0=0, nfeat=D, tag="n", bufs=None):
    if bufs is None:
        bufs = {}
    if "xt" not in bufs:
        bufs["xt"] = [k.sb("%s_xt%d" % (tag, i), [128, nfeat], F32) for i in range(2)]
        bufs["hb"] = [k.sb("%s_hb%d" % (tag, i), [128, nfeat], BF16) for i in range(2)]
        bufs["junk"] = k.sb(tag + "_junk", [128, nfeat], BF16)
        bufs["ss"] = [k.sb("%s_ss%d" % (tag, i), [128, 1], F32) for i in range(2)]
        bufs["rs"] = [k.sb("%s_rs%d" % (tag, i), [128, 1], F32) for i in range(2)]
        bufs["ptr"] = [k.ps("%s_ptr%d" % (tag, i), [128, 4, 128], BF16) for i in range(2)]
    xt, hb, junk, ss, rs, ptr = bufs["xt"], bufs["hb"], bufs["junk"], bufs["ss"], bufs["rs"], bufs["ptr"]
    nkc = nfeat // 128
    for t in range(ntiles):
        x_, h_, s_, r_ = xt[t % 2], hb[t % 2], ss[t % 2], rs[t % 2]
        k.dma("sp", x_[:], x_ap[t * 128:(t + 1) * 128, :], writes=[x_])
        k.op("act", lambda e, x_=x_, s_=s_: e.activation(out=junk[:], in_=x_[:], func=AF.Square, accum_out=s_[:]),
             reads=[x_], writes=[junk, s_])
        k.op("dve", lambda e, s_=s_, r_=r_: e.tensor_scalar(r_[:], s_[:], 1.0 / nfeat, EPS, op0=ALU.mult, op1=ALU.add),
             reads=[s_], writes=[r_])
        k.op("act", lambda e, r_=r_: e.activation(out=r_[:], in_=r_[:], func=AF.Sqrt), reads=[r_], writes=[r_])
        k.op("dve", lambda e, r_=r_: e.reciprocal(r_[:], r_[:]), reads=[r_], writes=[r_])
        k.op("dve", lambda e, x_=x_, h_=h_, r_=r_: e.scalar_tensor_tensor(out=h_[:], in0=x_[:], scalar=r_[:, 0:1], in1=gb[:],
                                                                          op0=ALU.mult, op1=ALU.mult),
             reads=[x_, r_, gb], writes=[h_])
        for q in range(nkc // 4):
            p_ = ptr[(t * (nkc // 4) + q) % 2]
            for j in range(4):
                kc = q * 4 + j
                k.op("pe", lambda e, p_=p_, h_=h_, kc=kc, j=j: e.transpose(p_[:, j, :], h_[:, kc * 128:(kc + 1) * 128], ident[:]),
                     reads=[h_, ident], writes=[p_])
            eng = "act" if q % 2 == 0 else "dve"
            dst = hT[:, q * 4:(q + 1) * 4, tcol0 + t * 128: tcol0 + (t + 1) * 128]
            if eng == "act":
                k.op("act", lambda e, dst=dst, p_=p_: e.copy(dst, p_[:]), reads=[p_], writes=[hT])
            else:
                k.op("dve", lambda e, dst=dst, p_=p_: e.tensor_copy(dst, p_[:]), reads=[p_], writes=[hT])


def wload(k, buf, pieces, c0, n):
    kc0 = 0
    for (w_ap, r0, nkc) in pieces:
        src = w_ap[r0:r0 + nkc * 128, c0:c0 + n].rearrange("(kc p) c -> p kc c", p=128)
        k.dma("pool", buf[:, kc0:kc0 + nkc, 0:n], src, writes=[buf])
        kc0 += nkc


def p1_body(k, c, x, w_in, gb_d, kvg_d, wuk_d, wuv_d, outs, tok):
    ident = c["ident"]
    gb = k.sb("gb_s", [128, D], F32)
    k.dma("sp", gb[:], gb_d[:, :], writes=[gb])
    kvg = k.sb("kvg_s", [128, 256], F32)
    k.dma("sp", kvg[:], kvg_d[:, :], writes=[kvg])
    hTb = [k.sb("hT%d" % i, [128, 16, 512], BF16) for i in range(T // 512)]
    nbufs = {}
    for tb_ in range(T // 512):
        rmsnorm_to_hT(k, x[tok + tb_ * 512: tok + (tb_ + 1) * 512, :], gb, hTb[tb_], ident, 4, bufs=nbufs)

    wbufs = [k.sb("win_wb%d" % i, [128, 16, 256], BF16) for i in range(3)]
    wcnt = [0]

    def wl_load(c0, n):
        b_ = wbufs[wcnt[0] % 3]
        wcnt[0] += 1
        wload(k, b_, [(w_in, 0, 16)], c0, n)
        return b_
    pacc = [k.ps("pacc%d" % i, [128, 512], F32) for i in range(5)]
    pi = [0]

    def next_ps():
        p = pacc[pi[0] % 5]
        pi[0] += 1
        return p

    fm_st = {F32: [k.sb("fmst_f%d" % i, [128, T], F32) for i in range(2)],
             BF16: [k.sb("fmst_b%d" % i, [128, T], BF16) for i in range(2)]}
    tm_st = {F32: [k.sb("tmst_f%d" % i, [128, 16, 24], F32) for i in range(2)],
             BF16: [k.sb("tmst_b%d" % i, [128, 16, 256], BF16) for i in range(2)]}
    cnt = {"fm": 0, "tm": 0, "ev": 0}

    def evac(dst_ap, dst_buf, p_, src_ap, act=None):
        if act == "sig":
            k.op("act", lambda e: e.activation(out=dst_ap, in_=src_ap, func=AF.Sigmoid), reads=[p_], writes=[dst_buf])
            return
        cnt["ev"] += 1
        if cnt["ev"] % 2 == 0:
            k.op("act", lambda e: e.copy(dst_ap, src_ap), reads=[p_], writes=[dst_buf])
        else:
            k.op("dve", lambda e: e.tensor_copy(dst_ap, src_ap), reads=[p_], writes=[dst_buf])

    def fm_group(name, c0, n, dt, act, row0):
        wb = wl_load(c0, n)
        M = min(128, n)
        for ch in range(n // M):
            st = fm_st[dt][cnt["fm"] % 2]
            cnt["fm"] += 1
            for tb in range(T // 512):
                p_ = next_ps()
                for kc in range(16):
                    k.op("pe", lambda e, p_=p_, kc=kc, ch=ch, tb=tb, wb=wb: e.matmul(
                        p_[0:M, :], lhsT=wb[:, kc, ch * M:(ch + 1) * M], rhs=hTb[tb][:, kc, :],
                        start=(kc == 0), stop=(kc == 15)), reads=[wb, hTb[tb]], writes=[p_])
                evac(st[0:M, tb * 512:(tb + 1) * 512], st, p_, p_[0:M, :], act)
            k.dma("sp", outs[name][row0 + ch * M: row0 + (ch + 1) * M, tok:tok + T], st[0:M, :], reads=[st])

    def tm_group(name, c0, n, dt, col0):
        wb = wl_load(c0, n)
        st = tm_st[dt][cnt["tm"] % 2]
        cnt["tm"] += 1
        for t in range(T // 128):
            p_ = next_ps()
            for kc in range(16):
                k.op("pe", lambda e, p_=p_, kc=kc, t=t, wb=wb: e.matmul(
                    p_[:, 0:n], lhsT=hTb[t // 4][:, kc, (t % 4) * 128:(t % 4 + 1) * 128], rhs=wb[:, kc, 0:n],
                    start=(kc == 0), stop=(kc == 15)), reads=[wb, hTb[t // 4]], writes=[p_])
            evac(st[:, t, 0:n], st, p_, p_[:, 0:n])
        dst = outs[name][tok:tok + T, col0:col0 + n].rearrange("(t p) c -> p t c", p=128)
        k.dma("sp", dst, st[:, :, 0:n], reads=[st])

    def ckv_group(c0):
        wb = wl_load(c0, 256)
        wuk_s = k.sb("wuk_s", [128, 2, 128], F32)
        wuv_s = k.sb("wuv_s", [128, 2, 128], F32)
        wuk = k.sb("wuk_b", [128, 2, 128], BF16)
        wuv = k.sb("wuv_b", [128, 2, 128], BF16)
        k.dma("sp", wuk_s[:], wuk_d.rearrange("(kc p) c -> p kc c", p=128), writes=[wuk_s])
        k.dma("sp", wuv_s[:], wuv_d.rearrange("(kc p) c -> p kc c", p=128), writes=[wuv_s])
        k.op("pool", lambda e: e.tensor_copy(wuk[:], wuk_s[:]), reads=[wuk_s], writes=[wuk])
        k.op("pool", lambda e: e.tensor_copy(wuv[:], wuv_s[:]), reads=[wuv_s], writes=[wuv])
        cT = k.sb("ckvT", [128, 2, T], BF16)
        cf = [k.sb("ckv_f%d" % i, [128, 256], F32) for i in range(2)]
        cb = [k.sb("ckv_b%d" % i, [128, 256], BF16) for i in range(2)]
        cj = k.sb("ckv_j", [128, 256], BF16)
        css = [k.sb("ckv_ss%d" % i, [128, 1], F32) for i in range(2)]
        ptr = k.ps("ckv_ptr", [128, 2, 128], BF16)
        for t in range(T // 128):
            p_ = next_ps()
            f_, b_, s_ = cf[t % 2], cb[t % 2], css[t % 2]
            for kc in range(16):
                k.op("pe", lambda e, p_=p_, kc=kc, t=t: e.matmul(
                    p_[:, 0:256], lhsT=hTb[t // 4][:, kc, (t % 4) * 128:(t % 4 + 1) * 128], rhs=wb[:, kc, 0:256],
                    start=(kc == 0), stop=(kc == 15)), reads=[wb, hTb[t // 4]], writes=[p_])
            k.op("act", lambda e, p_=p_, f_=f_: e.copy(f_[:], p_[:, 0:256]), reads=[p_], writes=[f_])
            k.op("act", lambda e, f_=f_, s_=s_: e.activation(out=cj[:], in_=f_[:], func=AF.Square, accum_out=s_[:]),
                 reads=[f_], writes=[cj, s_])
            k.op("dve", lambda e, s_=s_: e.tensor_scalar(s_[:], s_[:], 1.0 / 256, EPS, op0=ALU.mult, op1=ALU.add),
                 reads=[s_], writes=[s_])
            k.op("act", lambda e, s_=s_: e.activation(out=s_[:], in_=s_[:], func=AF.Sqrt), reads=[s_], writes=[s_])
            k.op("dve", lambda e, s_=s_: e.reciprocal(s_[:], s_[:]), reads=[s_], writes=[s_])
            k.op("dve", lambda e, f_=f_, b_=b_, s_=s_: e.scalar_tensor_tensor(out=b_[:], in0=f_[:], scalar=s_[:, 0:1], in1=kvg[:],
                                                                              op0=ALU.mult, op1=ALU.mult),
                 reads=[f_, s_, kvg], writes=[b_])
            for j in range(2):
                k.op("pe", lambda e, b_=b_, j=j: e.transpose(ptr[:, j, :], b_[:, j * 128:(j + 1) * 128], ident[:]),
                     reads=[b_, ident], writes=[ptr])
            k.op("dve", lambda e, t=t: e.tensor_copy(cT[:, :, t * 128:(t + 1) * 128], ptr[:]), reads=[ptr], writes=[cT])
        st = fm_st[BF16][cnt["fm"] % 2]
        cnt["fm"] += 1
        for tb in range(T // 512):
            p_ = next_ps()
            for kc in range(2):
                k.op("pe", lambda e, p_=p_, kc=kc, tb=tb: e.matmul(p_[:, :], lhsT=wuk[:, kc, :], rhs=cT[:, kc, tb * 512:(tb + 1) * 512],
                                                                   start=(kc == 0), stop=(kc == 1)), reads=[wuk, cT], writes=[p_])
            evac(st[:, tb * 512:(tb + 1) * 512], st, p_, p_[:, :])
        k.dma("sp", outs["dkT"][:, tok:tok + T], st[:, :], reads=[st])
        st = tm_st[BF16][cnt["tm"] % 2]
        cnt["tm"] += 1
        for t in range(T // 128):
            p_ = next_ps()
            for kc in range(2):
                k.op("pe", lambda e, p_=p_, kc=kc, t=t: e.matmul(p_[:, 0:128], lhsT=cT[:, kc, t * 128:(t + 1) * 128], rhs=wuv[:, kc, :],
                                                                 start=(kc == 0), stop=(kc == 1)), reads=[wuv, cT], writes=[p_])
            evac(st[:, t, 0:128], st, p_, p_[:, 0:128])
        k.dma("sp", outs["dv"][tok:tok + T, :].rearrange("(t p) c -> p t c", p=128), st[:, :, 0:128], reads=[st])

    for (name, c0, n, mode, dt, act) in SEGS:
        if mode == "FM":
            for g0 in range(0, n, 256):
                gn = min(256, n - g0)
                fm_group(name, c0 + g0, gn, dt, act, g0)
        elif mode == "TM":
            for g0 in range(0, n, 256):
                gn = min(256, n - g0)
                tm_group(name, c0 + g0, gn, dt, g0)
        else:
            ckv_group(c0)


P1_OUT = [s[0] for s in SEGS if s[3] != "CKV"] + ["dkT", "dv"]


def consts_common(k):
    c = {}
    c["ident"], c["identf"] = make_identity(k)
    ones = k.sb("ones_bf", [128, 128], BF16, persist=True)
    k.op("pool", lambda e: e.memset(ones[:], 1.0), writes=[ones])
    c["ones"] = ones
    zeros = k.sb("zeros_bf", [128, 512], BF16, persist=True)
    k.op("pool", lambda e: e.memset(zeros[:], 0.0), writes=[zeros])
    c["zeros"] = zeros
    tf = k.sb("tri_f", [128, 128], F32, persist=True)
    tri = k.sb("triS", [128, 128], BF16, persist=True)
    k.op("pool", lambda e: e.memset(tf[:], 1.0), writes=[tf])
    k.op("pool", lambda e: e.affine_select(out=tf[:], in_=tf[:], pattern=[[-1, 128]], compare_op=ALU.is_gt, fill=0.0,
                                           base=0, channel_multiplier=1), reads=[tf], writes=[tf])
    k.op("dve", lambda e: e.tensor_copy(tri[:], tf[:]), reads=[tf], writes=[tri])
    c["triS"] = tri
    return c


def run_pipeline(stage_lists):
    n = len(stage_lists)
    if n == 0:
        return
    nst = max(len(x) for x in stage_lists)
    for it in range(n + nst - 1):
        for s_ in range(nst):
            b = it - s_
            if 0 <= b < n and s_ < len(stage_lists[b]) and stage_lists[b][s_] is not None:
                stage_lists[b][s_]()


def build_sb(k, c, qT_d, kT_d, v_d, oT_d, nheads=2):
    ident, ones, tri, zeros = c["ident"], c["ones"], c["triS"], c["zeros"]
    NS = 3
    qT = [k.sb("sb_qT%d" % i, [128, S], BF16) for i in range(2)]
    kT = [k.sb("sb_kT%d" % i, [128, S], BF16) for i in range(2)]
    V = [k.sb("sb_V%d" % i, [128, 32, 128], BF16) for i in range(2)]
    zp = [k.ps("sb_zp%d" % i, [128, 512], F32) for i in range(2)]
    atri = [k.ps("sb_atri%d" % i, [128, 512], F32) for i in range(2)]
    accp = k.ps("sb_accp", [128, 512], F32)
    po = [k.ps("sb_po%d" % i, [128, 4, 128], F32) for i in range(2)]
    ptr = k.ps("sb_ptr", [128, 4, 128], BF16)
    E = [k.sb("sb_E%d" % i, [128, 512], F32) for i in range(NS)]
    SPb = [k.sb("sb_SP%d" % i, [128, 512], BF16) for i in range(NS + 1)]
    T1 = [k.sb("sb_T1%d" % i, [128, 512], F32) for i in range(NS)]
    T2 = [k.sb("sb_T2%d" % i, [128, 512], F32) for i in range(NS)]
    T3 = [k.sb("sb_T3%d" % i, [128, 512], F32) for i in range(NS)]
    W = [k.sb("sb_W%d" % i, [128, 512], BF16) for i in range(NS)]
    osb = [k.sb("sb_osb%d" % i, [128, 4, 128], BF16) for i in range(2)]
    oTs = [k.sb("sb_oTs%d" % i, [128, 512], BF16) for i in range(2)]
    stages = []
    it = 0
    for h in range(nheads):
        qT_, kT_, V_ = qT[h % 2], kT[h % 2], V[h % 2]
        first_of_head = True
        for qb in range(S // 512):
            po_ = po[qb % 2]
            kts = list(reversed(range(4 * qb + 4)))
            for bi, kt in enumerate(kts):
                kk = kt - 4 * qb
                n0 = max(kk, 0) * 128
                i3, i2 = it % NS, it % 2
                it += 1
                zp_, at_ = zp[i2], atri[i2]
                E_, SP_, T1_, T2_, T3_, W_ = E[i3], SPb[(it - 1) % (NS + 1)], T1[i3], T2[i3], T3[i3], W[i3]
                cs = slice(n0, 512)
                ds = slice(n0, n0 + 128)
                first = (bi == 0)
                last = (bi == len(kts) - 1)
                prev_blk = None if first else prev_info
                prev_info = (SP_, cs)

                def st0(h=h, qb=qb, kt=kt, kk=kk, n0=n0, cs=cs, ds=ds, zp_=zp_, E_=E_, SP_=SP_, qT_=qT_, kT_=kT_, V_=V_, load=first_of_head):
                    if load:
                        k.dma("sp", qT_[:], qT_d[h * 128:(h + 1) * 128, :], writes=[qT_])
                        k.dma("sp", kT_[:], kT_d[h * 128:(h + 1) * 128, :], writes=[kT_])
                        k.dma("sp", V_[:], v_d[:, h * 128:(h + 1) * 128].rearrange("(t p) d -> p t d", p=128), writes=[V_])
                    k.op("pe", lambda e: e.matmul(zp_[:, cs], lhsT=kT_[:, kt * 128:(kt + 1) * 128], rhs=qT_[:, qb * 512 + n0:(qb + 1) * 512], start=True, stop=True),
                         reads=[kT_, qT_], writes=[zp_])
                    k.op("act", lambda e: e.activation(out=E_[:, cs], in_=zp_[:, cs], func=AF.Exp, scale=SCALE), reads=[zp_], writes=[E_])
                    k.op("act", lambda e: e.activation(out=SP_[:, cs], in_=E_[:, cs], func=AF.Ln, bias=1.0), reads=[E_], writes=[SP_])
                    if kk >= 0:
                        k.op("pool", lambda e: e.affine_select(out=SP_[:, ds], in_=SP_[:, ds], pattern=[[1, 128]], compare_op=ALU.is_gt, fill=0.0, base=0,
                                                               channel_multiplier=-1), reads=[SP_], writes=[SP_])

                def st1(kk=kk, cs=cs, ds=ds, zp_=zp_, at_=at_, SP_=SP_, T1_=T1_, T2_=T2_, T3_=T3_, W_=W_, first=first, prev=prev_blk):
                    k.op("pe", lambda e: e.matmul(at_[:, cs], lhsT=tri[:], rhs=SP_[:, cs], start=True, stop=True), reads=[tri, SP_], writes=[at_])
                    if first:
                        k.op("pe", lambda e: e.matmul(accp[:, :], lhsT=zeros[:, 0:128], rhs=zeros[:, :], start=True, stop=False), reads=[zeros], writes=[accp])
                    else:
                        pSP, pcs = prev
                        k.op("pe", lambda e: e.matmul(accp[:, pcs], lhsT=ones[:], rhs=pSP[:, pcs], start=False, stop=False), reads=[ones, pSP], writes=[accp])
                    k.op("dve", lambda e: e.scalar_tensor_tensor(out=T1_[:, cs], in0=zp_[:, cs], scalar=SCALE, in1=SP_[:, cs], op0=ALU.mult, op1=ALU.subtract),
                         reads=[zp_, SP_], writes=[T1_])
                    k.op("dve", lambda e: e.tensor_tensor(out=T2_[:, cs], in0=T1_[:, cs], in1=at_[:, cs], op=ALU.subtract), reads=[T1_, at_], writes=[T2_])
                    k.op("dve", lambda e: e.tensor_tensor(out=T3_[:, cs], in0=T2_[:, cs], in1=accp[:, cs], op=ALU.subtract), reads=[T2_, accp], writes=[T3_])
                    k.op("act", lambda e: e.activation(out=W_[:, cs], in_=T3_[:, cs], func=AF.Exp), reads=[T3_], writes=[W_])
                    if kk >= 0:
                        k.op("pool", lambda e: e.affine_select(out=W_[:, ds], in_=W_[:, ds], pattern=[[1, 128]], compare_op=ALU.is_gt, fill=0.0, base=0,
                                                               channel_multiplier=-1), reads=[W_], writes=[W_])

                def st2(h=h, qb=qb, kt=kt, kk=kk, W_=W_, V_=V_, po_=po_, first=first, last=last):
                    if first:
                        k.op("pe", lambda e: e.matmul(po_[:].rearrange("p a b -> p (a b)"), lhsT=zeros[:, 0:128], rhs=zeros[:, :], start=True, stop=False),
                             reads=[zeros], writes=[po_])
                    for j in range(max(kk, 0), 4):
                        k.op("pe", lambda e, j=j: e.matmul(po_[:, j, :], lhsT=W_[:, j * 128:(j + 1) * 128], rhs=V_[:, kt, :], start=False, stop=(kt == 0 and j == 3)),
                             reads=[W_, V_], writes=[po_])
                    if last:
                        o_ = osb[qb % 2]
                        oT_ = oTs[qb % 2]
                        k.op("act", lambda e: e.copy(o_[:], po_[:]), reads=[po_], writes=[o_])
                        for j in range(4):
                            k.op("pe", lambda e, j=j: e.transpose(ptr[:, j, :], o_[:, j, :], ident[:]), reads=[o_, ident], writes=[ptr])
                        k.op("dve", lambda e: e.tensor_copy(oT_[:], ptr[:].rearrange("p a b -> p (a b)")), reads=[ptr], writes=[oT_])
                        k.dma("pool", oT_d[h * 128:(h + 1) * 128, qb * 512:(qb + 1) * 512], oT_[:], reads=[oT_])

                stages.append([st0, st1, st2])
                first_of_head = False
    run_pipeline(stages)


def build_nsa(k, c, d, oaT_d):
    ident, zeros = c["ident"], c["zeros"]
    A = [k.ps("n_A%d" % i, [128, 512], F32) for i in range(2)]
    B = [k.ps("n_B%d" % i, [128, 512], F32) for i in range(2)]
    Cb = [k.ps("n_C%d" % i, [128, 2, 256], F32) for i in range(2)]
    Dt = k.ps("n_D", [128, 4, 128], BF16)
    qT = k.sb("n_qT", [128, 4, S], BF16)
    k.dma("sp", qT[:], d["qnT"].rearrange("(h e) t -> e h t", h=4), writes=[qT])
    ksT = k.sb("n_ksT", [128, S], BF16)
    kwT = k.sb("n_kwT", [128, S], BF16)
    k.dma("sp", ksT[:], d["ksT"][:, :], writes=[ksT])
    k.dma("sp", kwT[:], d["kwT"][:, :], writes=[kwT])
    V1s = k.sb("n_V1s", [128, 32, 129], BF16)
    V1w = k.sb("n_V1w", [128, 32, 129], BF16)
    for V1, nm in ((V1s, "vs"), (V1w, "vw")):
        k.op("pool", lambda e, V1=V1: e.memset(V1[:, :, 128:129], 1.0), writes=[V1])
        k.dma("sp", V1[:, :, 0:128], d[nm].rearrange("(t p) e -> p t e", p=128), writes=[V1])
    wslc = k.sb("n_wslc", [128, 4, 1024], F32)
    wwin = k.sb("n_wwin", [128, 4, 1408], F32)
    mext = k.sb("n_mext", [128, 4, 503], F32)
    nc31 = k.sb("n_c31", [128, 4], F32)
    keepm = k.sb("n_keep", [128, 128], F32)
    addm = k.sb("n_add", [128, 128], F32)
    ovb = k.sb("n_ov", [128, 2, 64], BF16)
    eexp = k.sb("n_eexp", [64, S], BF16)
    for t_, nm in ((wslc, "wslc"), (wwin, "wwin"), (mext, "mext"), (nc31, "nc31"), (keepm, "keep"), (addm, "add"), (ovb, "ov"), (eexp, "eexp")):
        k.dma("sp", t_[:], d[nm], writes=[t_])
    ng = k.sb("n_ng", [128, 32, 12], F32)
    sg = k.sb("n_sg", [128, 32, 12], F32)
    k.dma("sp", ng[:], d["ng"].rearrange("(t p) c -> p t c", p=128), writes=[ng])
    k.op("act", lambda e: e.activation(out=sg[:], in_=ng[:], func=AF.Sigmoid), reads=[ng], writes=[sg])
    kcTc = k.sb("n_kcTc", [128, 256], BF16)
    vc = k.sb("n_vc", [128, 2, 128], BF16)
    k.op("pool", lambda e: e.memset(vc[:], 0.0), writes=[vc])

    st = ExitStack()
    aT = k.sb("n_aT", [128, S], BF16, stack=st)
    w1s = [k.sb("n_w1s%d" % i, [128, 8, 256], F32, stack=st) for i in range(2)]
    w1b = k.sb("n_w1b", [128, 32, 256], BF16, stack=st)
    w2s = k.sb("n_w2s", [128, 2, 128], F32, stack=st)
    w2b = k.sb("n_w2b", [128, 2, 128], BF16, stack=st)
    pes = k.sb("n_pes", [128, 32], F32, stack=st)
    peb = k.sb("n_peb", [128, 32], BF16, stack=st)
    pbias = k.sb("n_pbias", [128, 2], F32, stack=st)
    gT = k.sb("n_gT", [128, 2, 256], BF16, stack=st)
    k.op("pool", lambda e: e.memset(gT[:], 0.0), writes=[gT])
    for which in ("k", "v"):
        k.dma("sp", aT[:], d["kcT" if which == "k" else "vcT"][:, :], writes=[aT])
        w1_d, w2_d, pe_d = d["c%s_w1" % which], d["c%s_w2" % which], d["c%s_peT" % which]
        for q in range(4):
            s_ = w1s[q % 2]
            k.dma("sp", s_[:], w1_d[q * 1024:(q + 1) * 1024, :].rearrange("(p e) h -> e p h", e=128), writes=[s_])
            k.op("pool", lambda e, s_=s_, q=q: e.tensor_copy(w1b[:, q * 8:(q + 1) * 8, :], s_[:]), reads=[s_], writes=[w1b])
        k.dma("sp", w2s[:], w2_d.rearrange("(c p) e -> p c e", p=128), writes=[w2s])
        k.op("pool", lambda e: e.tensor_copy(w2b[:], w2s[:]), reads=[w2s], writes=[w2b])
        k.dma("sp", pes[:], pe_d[:, :], writes=[pes])
        k.op("pool", lambda e: e.tensor_copy(peb[:], pes[:]), reads=[pes], writes=[peb])
        for hc in range(2):
            pb = B[hc]
            for p in range(32):
                k.op("pe", lambda e, pb=pb, p=p, hc=hc: e.matmul(pb[:, 0:1], lhsT=w1b[:, p, hc * 128:(hc + 1) * 128], rhs=peb[:, p:p + 1],
                                                               start=(p == 0), stop=(p == 31)), reads=[w1b, peb], writes=[pb])
            k.op("dve", lambda e, pb=pb, hc=hc: e.tensor_copy(pbias[:, hc:hc + 1], pb[:, 0:1]), reads=[pb], writes=[pbias])
            ph = A[hc]
            for p in range(32):
                k.op("pe", lambda e, ph=ph, p=p, hc=hc: e.matmul(ph[:, 0:255], lhsT=w1b[:, p, hc * 128:(hc + 1) * 128], rhs=aT[:, p:p + 4065:16],
                                                               start=(p == 0), stop=(p == 31)), reads=[w1b, aT], writes=[ph])
            k.op("act", lambda e, ph=ph, hc=hc: e.activation(out=gT[:, hc, 0:255], in_=ph[:, 0:255], func=AF.Gelu_apprx_tanh, bias=pbias[:, hc:hc + 1]),
                 reads=[ph, pbias], writes=[gT])
        if which == "k":
            po_ = B[0]
            for hc in range(2):
                k.op("pe", lambda e, hc=hc, po_=po_: e.matmul(po_[:, 0:255], lhsT=w2b[:, hc, :], rhs=gT[:, hc, 0:255], start=(hc == 0), stop=(hc == 1)),
                     reads=[w2b, gT], writes=[po_])
            k.op("pool", lambda e: e.memset(kcTc[:], 0.0), writes=[kcTc])
            k.op("act", lambda e, po_=po_: e.copy(kcTc[:, 0:255], po_[:, 0:255]), reads=[po_], writes=[kcTc])
        else:
            for ch in range(2):
                rows = 128 if ch == 0 else 127
                po_ = B[ch]
                for hc in range(2):
                    k.op("pe", lambda e, hc=hc, ch=ch, rows=rows, po_=po_: e.matmul(po_[0:rows, 0:128], lhsT=gT[:, hc, ch * 128:ch * 128 + rows], rhs=w2b[:, hc, :],
                                                                                 start=(hc == 0), stop=(hc == 1)), reads=[w2b, gT], writes=[po_])
                k.op("act", lambda e, ch=ch, rows=rows, po_=po_: e.copy(vc[0:rows, ch, :], po_[0:rows, 0:128]), reads=[po_], writes=[vc])
    k.barrier()
    st.close()

    Lc = k.sb("n_Lc", [128, 4, 256], F32)
    ec = k.sb("n_ec", [128, 4, 256], F32)
    pbf = k.sb("n_pbf", [128, 4, 256], BF16)
    k.op("pool", lambda e: e.memset(pbf[:], 0.0), writes=[pbf])
    pT = k.sb("n_pT", [128, 4, 2, 128], BF16)
    mx = k.sb("n_mx", [128, 4], F32)
    Zc = k.sb("n_Zc", [128, 4], F32)
    scs = k.sb("n_scs", [128, 64], F32)
    swork = k.sb("n_swork", [128, 64], F32)
    m8a = k.sb("n_m8a", [128, 8], F32)
    m8b = k.sb("n_m8b", [128, 8], F32)
    sel = k.sb("n_sel", [128, 64], BF16)
    selT = k.sb("n_selT", [64, 512], BF16)
    Oacc = k.sb("n_Oacc", [128, 4, 4, 128], F32)
    Lg = [k.sb("n_Lg%d" % i, [128, 512], F32) for i in range(3)]
    Eb = [k.sb("n_E%d" % i, [128, 512], F32) for i in range(3)]
    PT = [k.sb("n_PT%d" % i, [128, 512], BF16) for i in range(5)]
    rz = k.sb("n_rz", [128, 1], F32)
    maskS = k.sb("n_maskS", [128, 32, 512], BF16)
    ocp = [k.sb("n_ocp%d" % i, [128, 2, 2, 256], F32) for i in range(1)]
    obf = k.sb("n_obf", [128, 4, 4, 128], BF16)
    oTs = [k.sb("n_oTs%d" % i, [128, 4, 128], BF16) for i in range(2)]
    ci = [0]

    def attn(qb, h, br):
        kT, V1, wide, gcol = (ksT, V1s, wslc, 1) if br == "slc" else (kwT, V1w, wwin, 2)
        for cb in Cb:
            k.op("pe", lambda e, cb=cb: e.matmul(cb[:].rearrange("p a b -> p (a b)"), lhsT=zeros[:, 0:128], rhs=zeros[:, :], start=True, stop=False),
                 reads=[zeros], writes=[cb])
        kts = list(range(0, 4 * qb + 4)) if br == "slc" else list(range(max(0, 4 * qb - 4), 4 * qb + 4))
        stages = []
        for kt in kts:
            kk = kt - 4 * qb
            jlo = max(kk, 0)
            jhi = 3 if br == "slc" else min(3, kk + 4)
            cs = slice(jlo * 128, (jhi + 1) * 128)
            i_ = ci[0] % 2
            i3 = ci[0] % 3
            i5 = ci[0] % 5
            ci[0] += 1
            ps_, Lg_, E_, PT_, pm_ = A[i_], Lg[i3], Eb[i3], PT[i5], B[i_]

            def st0(kt=kt, kk=kk, jlo=jlo, jhi=jhi, cs=cs, ps_=ps_, Lg_=Lg_, E_=E_, PT_=PT_, pm_=pm_):
                k.op("pe", lambda e: e.matmul(ps_[:, cs], lhsT=kT[:, kt * 128:(kt + 1) * 128], rhs=qT[:, h, qb * 512 + jlo * 128: qb * 512 + (jhi + 1) * 128],
                                              start=True, stop=True), reads=[kT, qT], writes=[ps_])
                near = (kk >= -1) if br == "slc" else True
                eout = E_ if br == "slc" else PT_
                if near:
                    w0 = (3 - kk) * 128
                    k.op("dve", lambda e: e.scalar_tensor_tensor(out=Lg_[:, cs], in0=ps_[:, cs], scalar=SCALE, in1=wide[:, h, w0 + jlo * 128: w0 + (jhi + 1) * 128],
                                                                 op0=ALU.mult, op1=ALU.add), reads=[ps_, wide], writes=[Lg_])
                    k.op("act", lambda e: e.activation(out=eout[:, cs], in_=Lg_[:, cs], func=AF.Exp), reads=[Lg_], writes=[eout])
                else:
                    k.op("act", lambda e: e.activation(out=eout[:, cs], in_=ps_[:, cs], func=AF.Exp, scale=SCALE, bias=nc31[:, h:h + 1]), reads=[ps_, nc31], writes=[eout])

            def st0b(cs=cs, E_=E_, PT_=PT_, kt=kt):
                k.op("dve", lambda e: e.tensor_tensor(out=PT_[:, cs], in0=E_[:, cs], in1=maskS[:, kt, cs], op=ALU.mult), reads=[E_, maskS], writes=[PT_])

            def st1(kt=kt, kk=kk, jlo=jlo, jhi=jhi, PT_=PT_):
                for j in range(jlo, jhi + 1):
                    cb = Cb[j // 2]
                    k.op("pe", lambda e, j=j, cb=cb: e.matmul(cb[:, j % 2, 0:129], lhsT=PT_[:, j * 128:(j + 1) * 128], rhs=V1[:, kt, :],
                                                              start=False, stop=(kk == j and j % 2 == 1)), reads=[PT_, V1], writes=[cb])

            stages.append([st0, st0b if br == "slc" else None, None, st1])
        run_pipeline(stages)
        oc_ = ocp[0]
        for b_ in range(2):
            k.op("act", lambda e, b_=b_: e.copy(oc_[:, b_, :, :], Cb[b_][:]), reads=[Cb[b_]], writes=[oc_])
        for j in range(4):
            qt = 4 * qb + j
            k.op("dve", lambda e, j=j: e.reciprocal(rz[:], oc_[:, j // 2, j % 2, 128:129]), reads=[oc_], writes=[rz])
            k.op("dve", lambda e, qt=qt: e.tensor_scalar(rz[:], rz[:], sg[:, qt, 3 * h + gcol:3 * h + gcol + 1], None, op0=ALU.mult), reads=[rz, sg], writes=[rz])
            k.op("dve", lambda e, j=j: e.scalar_tensor_tensor(out=Oacc[:, j, h, :], in0=oc_[:, j // 2, j % 2, 0:128], scalar=rz[:, 0:1], in1=Oacc[:, j, h, :],
                                                             op0=ALU.mult, op1=ALU.add), reads=[oc_, rz, Oacc], writes=[Oacc])

    for qb in range(S // 512):
        for j in range(4):
            qt = 4 * qb + j
            for h in range(4):
                pl = A[h // 2]
                k.op("pe", lambda e, pl=pl, h=h, qt=qt: e.matmul(pl[:, (h % 2) * 256:(h % 2) * 256 + 255], lhsT=qT[:, h, qt * 128:(qt + 1) * 128], rhs=kcTc[:, 0:255],
                                                               start=True, stop=True), reads=[qT, kcTc], writes=[pl])
            for h in range(4):
                pl = A[h // 2]
                k.op("dve", lambda e, pl=pl, h=h, qt=qt: e.scalar_tensor_tensor(out=Lc[:, h, 0:255], in0=pl[:, (h % 2) * 256:(h % 2) * 256 + 255], scalar=SCALE,
                                                                              in1=mext[:, h, 248 - 8 * qt: 503 - 8 * qt], op0=ALU.mult, op1=ALU.add),
                     reads=[pl, mext], writes=[Lc])
            k.op("dve", lambda e: e.tensor_reduce(out=mx[:], in_=Lc[:, :, 0:255], axis=AX.X, op=ALU.max), reads=[Lc], writes=[mx])
            k.op("dve", lambda e: e.tensor_scalar(mx[:], mx[:], -100.0, -1.0, op0=ALU.max, op1=ALU.mult), reads=[mx], writes=[mx])
            for h in range(4):
                k.op("act", lambda e, h=h: e.activation(out=ec[:, h, 0:255], in_=Lc[:, h, 0:255], func=AF.Exp, bias=mx[:, h:h + 1], accum_out=Zc[:, h:h + 1]),
                     reads=[Lc, mx], writes=[ec, Zc])
            k.op("dve", lambda e: e.tensor_scalar(Zc[:], Zc[:], 1e-30, None, op0=ALU.max), reads=[Zc], writes=[Zc])
            k.op("dve", lambda e: e.reciprocal(Zc[:], Zc[:]), reads=[Zc], writes=[Zc])
            for h in range(4):
                eng = "dve" if h % 2 == 0 else "pool"
                k.op(eng, lambda e, h=h: e.tensor_scalar(pbf[:, h, 0:255], ec[:, h, 0:255], Zc[:, h:h + 1], None, op0=ALU.mult), reads=[ec, Zc], writes=[pbf])
            for h in range(4):
                for ch in range(2):
                    k.op("pe", lambda e, h=h, ch=ch: e.transpose(Dt[:, ch, :], pbf[:, h, ch * 128:(ch + 1) * 128], ident[:]), reads=[pbf, ident], writes=[Dt])
                if h % 2 == 0:
                    k.op("act", lambda e, h=h: e.copy(pT[:, h, :, :], Dt[:, 0:2, :]), reads=[Dt], writes=[pT])
                else:
                    k.op("dve", lambda e, h=h: e.tensor_copy(pT[:, h, :, :], Dt[:, 0:2, :]), reads=[Dt], writes=[pT])
            pcmp = B[0]
            pimp = B[1]
            for h in range(4):
                for ch in range(2):
                    k.op("pe", lambda e, h=h, ch=ch: e.matmul(pcmp[:, h * 128:(h + 1) * 128], lhsT=pT[:, h, ch, :], rhs=vc[:, ch, :], start=(ch == 0), stop=(ch == 1)),
                         reads=[pT, vc], writes=[pcmp])
            for h in range(4):
                for ch in range(2):
                    k.op("pe", lambda e, h=h, ch=ch: e.matmul(pimp[:, 0:64], lhsT=pT[:, h, ch, :], rhs=ovb[:, ch, :], start=(h == 0 and ch == 0), stop=(h == 3 and ch == 1)),
                         reads=[pT, ovb], writes=[pimp])
            for h in range(4):
                k.op("dve", lambda e, h=h, j=j, qt=qt: e.tensor_scalar(Oacc[:, j, h, :], pcmp[:, h * 128:(h + 1) * 128], sg[:, qt, 3 * h:3 * h + 1], None, op0=ALU.mult),
                     reads=[pcmp, sg], writes=[Oacc])
            m0 = 64 - 2 * qt
            k.op("dve", lambda e, m0=m0: e.tensor_tensor(out=scs[:], in0=pimp[:, 0:64], in1=keepm[:, m0:m0 + 64], op=ALU.mult), reads=[pimp, keepm], writes=[scs])
            k.op("dve", lambda e, m0=m0: e.tensor_tensor(out=scs[:], in0=scs[:], in1=addm[:, m0:m0 + 64], op=ALU.add), reads=[scs, addm], writes=[scs])
            k.op("dve", lambda e: e.memset(scs[:, 0:1], 3e9), writes=[scs])
            k.op("dve", lambda e: e.max(out=m8a[:], in_=scs[:]), reads=[scs], writes=[m8a])
            k.op("dve", lambda e: e.match_replace(out=swork[:], in_to_replace=m8a[:], in_values=scs[:], imm_value=-2.0), reads=[scs, m8a], writes=[swork])
            k.op("dve", lambda e: e.max(out=m8b[:], in_=swork[:]), reads=[swork], writes=[m8b])
            k.op("dve", lambda e: e.tensor_scalar(sel[:], scs[:], m8b[:, 7:8], None, op0=ALU.is_ge), reads=[scs, m8b], writes=[sel])
            k.op("pe", lambda e: e.transpose(Dt[0:64, 2, :], sel[:], ident[:]), reads=[sel, ident], writes=[Dt])
            k.op("act", lambda e, j=j: e.copy(selT[:, j * 128:(j + 1) * 128], Dt[0:64, 2, :]), reads=[Dt], writes=[selT])
        for kt in range(4 * qb + 4):
            jlo = max(kt - 4 * qb, 0)
            cs = slice(jlo * 128, 512)
            pm_ = B[kt % 2]
            k.op("pe", lambda e, pm_=pm_, kt=kt, cs=cs: e.matmul(pm_[:, cs], lhsT=eexp[:, kt * 128:(kt + 1) * 128], rhs=selT[:, cs], start=True, stop=True),
                 reads=[eexp, selT], writes=[pm_])
            k.op("act", lambda e, pm_=pm_, kt=kt, cs=cs: e.copy(maskS[:, kt, cs], pm_[:, cs]), reads=[pm_], writes=[maskS])
        for h in range(4):
            attn(qb, h, "slc")
            attn(qb, h, "win")
        k.op("act", lambda e: e.copy(obf[:], Oacc[:]), reads=[Oacc], writes=[obf])
        for h in range(4):
            oT_ = oTs[h % 2]
            for j in range(4):
                k.op("pe", lambda e, h=h, j=j: e.transpose(Dt[:, j, :], obf[:, j, h, :], ident[:]), reads=[obf, ident], writes=[Dt])
            k.op("dve", lambda e, oT_=oT_: e.tensor_copy(oT_[:], Dt[:]), reads=[Dt], writes=[oT_])
            k.dma("pool", oaT_d[h * 128:(h + 1) * 128, qb * 512:(qb + 1) * 512], oT_[:].rearrange("p a b -> p (a b)"), reads=[oT_])


def build_dsa(k, c, dqT_d, dkT_d, dv_d, iqT_d, ikT_d, iw_d, bias_d, c31_d, caus_d, ocT_d, nqt=16, near=3, parity=True):
    ident = c["ident"]
    WSC = (8 ** -0.5) * (64 ** -0.5)
    CW = (near - 1) * 128
    dkT = k.sb("d_kT", [128, S], BF16)
    V1 = k.sb("d_V1", [128, 32, 129], BF16)
    ikT = k.sb("d_ikT", [64, S], BF16)
    bias = k.sb("d_bias", [128, 4, near, 128], F32)
    caus = k.sb("d_caus", [128, CW], F32)
    c31 = k.sb("d_c31", [128, 4], F32)
    k.dma("sp", caus[:], caus_d[:, :], writes=[caus])
    k.dma("sp", dkT[:], dkT_d[:, :], writes=[dkT])
    k.op("pool", lambda e: e.memset(V1[:, :, 128:129], 1.0), writes=[V1])
    k.dma("sp", V1[:, :, 0:128], dv_d.rearrange("(t p) d -> p t d", p=128), writes=[V1])
    k.dma("sp", ikT[:], ikT_d[:, :], writes=[ikT])
    k.dma("sp", bias[:], bias_d[:, :, :, :], writes=[bias])
    k.dma("sp", c31[:], c31_d[:, :], writes=[c31])
    dq = [k.sb("d_q%d" % i, [128, 4, 128], BF16) for i in range(2)]
    iq = [k.sb("d_iq%d" % i, [64, 8, 128], BF16) for i in range(2)]
    iw = [k.sb("d_iw%d" % i, [128, 8], F32) for i in range(2)]
    absw = [k.sb("d_absw%d" % i, [128, 8], F32) for i in range(2)]
    sgn = [k.sb("d_sgn%d" % i, [128, 8], F32) for i in range(2)]
    sc = [k.sb("d_sc%d" % i, [128, S], F32) for i in range(2)]
    jk1 = [k.sb("d_jk1%d" % i, [128, S], BF16) for i in range(2)]
    jk2 = [k.sb("d_jk2%d" % i, [128, S], BF16) for i in range(2)]
    sv = [{nm: k.sb("d_%s%d" % (nm, i), [128, 2 if nm == "cnt2" else 1], F32) for nm in ("lo", "hi", "w0", "t1", "nt2", "cnt2", "asum")} for i in range(2)]
    mask = k.sb("d_mask", [128, S], BF16)
    maskT = k.sb("d_maskT", [128, 32, 128], BF16)
    R = [k.sb("d_R%d" % i, [128, 512], F32) for i in range(3)]
    E = [k.sb("d_E%d" % i, [128, 4, 128], F32) for i in range(3)]
    Lg = [k.sb("d_Lg%d" % i, [128, 128], F32) for i in range(3)]
    PT = [k.sb("d_PT%d" % i, [128, 4, 128], BF16) for i in range(4)]
    rz = k.sb("d_rz", [128, 1], F32)
    osb = [k.sb("d_osb%d" % i, [128, 4, 128], BF16) for i in range(2)]
    oTs = [k.sb("d_oTs%d" % i, [128, 4, 128], BF16) for i in range(2)]
    pdot = [k.ps("d_pdot%d" % i, [128, 512], F32) for i in range(2)]
    pst = [k.ps("d_pst%d" % i, [128, 4, 128], F32) for i in range(2)]
    po = [k.ps("d_po%d" % i, [128, 129], F32) for i in range(2)]
    ptr = [k.ps("d_ptr%d" % i, [128, 4, 128], BF16) for i in range(2)]
    cn = {"R": 0, "S": 0, "P": 0}

    def indexer(qi, t):
        qt = (2 * qi + 1) if parity else qi
        L = (qt + 1) * 128
        dq_, iq_, iw_, aw_, sg_, sc_ = dq[t], iq[t], iw[t], absw[t], sgn[t], sc[t]
        k.dma("sp", dq_[:], dqT_d[:, qi * 128:(qi + 1) * 128].rearrange("(h d) t -> d h t", h=4), writes=[dq_])
        k.dma("sp", iq_[:], iqT_d[:, qi * 128:(qi + 1) * 128].rearrange("(h e) t -> e h t", h=8), writes=[iq_])
        k.dma("sp", iw_[:], iw_d[qi * 128:(qi + 1) * 128, :], writes=[iw_])
        k.op("act", lambda e: e.activation(out=aw_[:], in_=iw_[:], func=AF.Abs, scale=WSC), reads=[iw_], writes=[aw_])
        k.op("act", lambda e: e.activation(out=sg_[:], in_=iw_[:], func=AF.Sign), reads=[iw_], writes=[sg_])
        for sb_ in range((L + 511) // 512):
            w = min(512, L - sb_ * 512)
            for h in range(8):
                pd = pdot[cn["R"] % 2]
                R_ = R[cn["R"] % 3]
                cn["R"] += 1
                k.op("pe", lambda e, pd=pd, h=h, sb_=sb_, w=w: e.matmul(pd[:, 0:w], lhsT=iq_[:, h, :], rhs=ikT[:, sb_ * 512: sb_ * 512 + w], start=True, stop=True),
                     reads=[iq_, ikT], writes=[pd])
                k.op("act", lambda e, pd=pd, R_=R_, h=h, w=w: e.activation(out=R_[:, 0:w], in_=pd[:, 0:w], func=AF.Relu, scale=aw_[:, h:h + 1]),
                     reads=[pd, aw_], writes=[R_])
                scs = sc_[:, sb_ * 512: sb_ * 512 + w]
                if h == 0:
                    k.op("dve", lambda e, R_=R_, scs=scs, w=w, h=h: e.tensor_scalar(scs, R_[:, 0:w], sg_[:, h:h + 1], None, op0=ALU.mult), reads=[R_, sg_], writes=[sc_])
                else:
                    k.op("dve", lambda e, R_=R_, scs=scs, w=w, h=h: e.scalar_tensor_tensor(out=scs, in0=R_[:, 0:w], scalar=sg_[:, h:h + 1], in1=scs, op0=ALU.mult, op1=ALU.add),
                         reads=[R_, sg_, sc_], writes=[sc_])
        k.op("pool", lambda e: e.tensor_tensor(out=sc_[:, L - CW:L], in0=sc_[:, L - CW:L], in1=caus[:], op=ALU.add), reads=[sc_, caus], writes=[sc_])

    def topk_init(qi, t):
        qt = (2 * qi + 1) if parity else qi
        L = (qt + 1) * 128
        v, sc_ = sv[t], sc[t]
        k.op("dve", lambda e: e.tensor_reduce(out=v["lo"][:], in_=sc_[:, 0:L - CW], axis=AX.X, op=ALU.min), reads=[sc_], writes=[v["lo"]])
        k.op("dve", lambda e: e.tensor_reduce(out=v["hi"][:], in_=sc_[:, 0:L], axis=AX.X, op=ALU.max), reads=[sc_], writes=[v["hi"]])
        k.op("dve", lambda e: e.tensor_tensor(out=v["w0"][:], in0=v["hi"][:], in1=v["lo"][:], op=ALU.subtract), reads=[v["hi"], v["lo"]], writes=[v["w0"]])

    def topk_iter(qi, t, itx):
        qt = (2 * qi + 1) if parity else qi
        L = (qt + 1) * 128
        v, sc_, j1, j2 = sv[t], sc[t], jk1[t], jk2[t]
        lo, w0, t1, nt2, cnt2, asum = v["lo"], v["w0"], v["t1"], v["nt2"], v["cnt2"], v["asum"]
        cf = 3.0 ** -(itx + 1)
        k.op("dve", lambda e: e.tensor_scalar(t1[:], w0[:], cf, lo[:, 0:1], op0=ALU.mult, op1=ALU.add), reads=[w0, lo], writes=[t1])
        k.op("dve", lambda e: e.tensor_scalar(nt2[:], w0[:], -2.0 * cf, lo[:, 0:1], op0=ALU.mult, op1=ALU.subtract), reads=[w0, lo], writes=[nt2])
        k.op("dve", lambda e: e.tensor_scalar(j1[:, 0:L], sc_[:, 0:L], t1[:, 0:1], 0.0, op0=ALU.is_ge, op1=ALU.add, accum_out=cnt2[:, 0:1]),
             reads=[sc_, t1, cnt2], writes=[j1, cnt2])
        k.op("act", lambda e: e.activation(out=j2[:, 0:L], in_=sc_[:, 0:L], func=AF.Sign, bias=nt2[:, 0:1], accum_out=cnt2[:, 1:2]),
             reads=[sc_, nt2, cnt2], writes=[j2, cnt2])
        k.op("dve", lambda e: e.tensor_scalar(asum[:], cnt2[:, 0:1], 255.5, None, op0=ALU.is_ge), reads=[cnt2], writes=[asum])
        k.op("dve", lambda e: e.scalar_tensor_tensor(out=asum[:], in0=cnt2[:, 1:2], scalar=511.5 - L, in1=asum[:], op0=ALU.is_ge, op1=ALU.add),
             reads=[cnt2, asum], writes=[asum])
        k.op("dve", lambda e: e.tensor_tensor(out=asum[:], in0=asum[:], in1=w0[:], op=ALU.mult), reads=[asum, w0], writes=[asum])
        k.op("dve", lambda e: e.scalar_tensor_tensor(out=lo[:], in0=asum[:], scalar=cf, in1=lo[:], op0=ALU.mult, op1=ALU.add), reads=[asum, lo], writes=[lo])

    def attend(qi, t):
        qt = (2 * qi + 1) if parity else qi
        L = (qt + 1) * 128
        sc_, dq_, v = sc[t], dq[t], sv[t]
        if qt >= 2:
            k.op("dve", lambda e: e.tensor_scalar(mask[:, 0:L], sc_[:, 0:L], v["lo"][:, 0:1], None, op0=ALU.is_ge), reads=[sc_, v["lo"]], writes=[mask])
        else:
            k.op("dve", lambda e: e.tensor_scalar(mask[:, 0:L], sc_[:, 0:L], -1e29, None, op0=ALU.is_ge), reads=[sc_], writes=[mask])
        for k0 in range(0, qt + 1, 4):
            nk = min(4, qt + 1 - k0)
            p_ = ptr[cn["P"] % 2]
            cn["P"] += 1
            for j in range(nk):
                k.op("pe", lambda e, p_=p_, j=j, k0=k0: e.transpose(p_[:, j, :], mask[:, (k0 + j) * 128:(k0 + j + 1) * 128], ident[:]), reads=[mask, ident], writes=[p_])
            k.op("act", lambda e, p_=p_, k0=k0, nk=nk: e.copy(maskT[:, k0:k0 + nk, :], p_[:, 0:nk, :]), reads=[p_], writes=[maskT])
        o_ = osb[qi % 2]
        stages = []
        for h in range(4):
            po_ = po[h % 2]
            groups = []
            nfar = max(qt - near + 1, 0)
            for k0 in range(0, nfar, 4):
                groups.append(("far", k0, min(4, nfar - k0)))
            for kt in range(nfar, qt + 1):
                groups.append(("near", kt, 1))
            for gi, (kind, k0, nk) in enumerate(groups):
                ps_ = pst[cn["S"] % 2]
                E_ = E[cn["S"] % 3]
                PT_ = PT[cn["S"] % 4]
                Lg_ = Lg[cn["S"] % 3]
                eng = "pool" if cn["S"] % 2 == 0 else "dve"
                cn["S"] += 1
                lastg = (gi == len(groups) - 1)

                def st0(kind=kind, k0=k0, nk=nk, ps_=ps_, E_=E_, PT_=PT_, Lg_=Lg_, eng=eng, h=h):
                    for j in range(nk):
                        k.op("pe", lambda e, j=j: e.matmul(ps_[:, j, :], lhsT=dkT[:, (k0 + j) * 128:(k0 + j + 1) * 128], rhs=dq_[:, h, :], start=True, stop=True),
                             reads=[dkT, dq_], writes=[ps_])
                    if kind == "far":
                        k.op("act", lambda e: e.activation(out=E_[:, 0:nk, :], in_=ps_[:, 0:nk, :], func=AF.Exp, scale=SCALE, bias=c31[:, h:h + 1]),
                             reads=[ps_, c31], writes=[E_])
                    else:
                        bi = k0 - (qt - near + 1)
                        k.op("dve", lambda e: e.scalar_tensor_tensor(out=Lg_[:], in0=ps_[:, 0, :], scalar=SCALE, in1=bias[:, h, bi, :], op0=ALU.mult, op1=ALU.add),
                             reads=[ps_, bias], writes=[Lg_])
                        k.op("act", lambda e: e.activation(out=E_[:, 0, :], in_=Lg_[:], func=AF.Exp), reads=[Lg_], writes=[E_])

                def st0b(k0=k0, nk=nk, E_=E_, PT_=PT_, eng=eng):
                    k.op(eng, lambda e: e.tensor_tensor(out=PT_[:, 0:nk, :], in0=E_[:, 0:nk, :], in1=maskT[:, k0:k0 + nk, :], op=ALU.mult),
                         reads=[E_, maskT], writes=[PT_])

                def st1(k0=k0, nk=nk, PT_=PT_, po_=po_, h=h, lastg=lastg):
                    for j in range(nk):
                        k.op("pe", lambda e, j=j: e.matmul(po_[:, :], lhsT=PT_[:, j, :], rhs=V1[:, k0 + j, :], start=(k0 + j == 0), stop=(k0 + j == qt)),
                             reads=[PT_, V1], writes=[po_])
                    if lastg:
                        k.op("dve", lambda e: e.reciprocal(rz[:], po_[:, 128:129]), reads=[po_], writes=[rz])
                        k.op("dve", lambda e: e.tensor_scalar(o_[:, h, :], po_[:, 0:128], rz[:, 0:1], None, op0=ALU.mult), reads=[po_, rz], writes=[o_])

                stages.append([st0, st0b, None, st1])
        run_pipeline(stages)
        p_ = ptr[cn["P"] % 2]
        cn["P"] += 1
        oT_ = oTs[qi % 2]
        for h in range(4):
            k.op("pe", lambda e, p_=p_, h=h: e.transpose(p_[:, h, :], o_[:, h, :], ident[:]), reads=[o_, ident], writes=[p_])
        k.op("act", lambda e: e.copy(oT_[:], p_[:]), reads=[p_], writes=[oT_])
        k.dma("pool", ocT_d[:, qi * 128:(qi + 1) * 128].rearrange("(h d) t -> d h t", h=4), oT_[:], reads=[oT_])

    for q0 in range(0, nqt, 2):
        pair = [(q0 + t, t) for t in range(2) if q0 + t < nqt]
        for (qi, t) in pair:
            indexer(qi, t)
        need = [(qi, t) for (qi, t) in pair if ((2 * qi + 1) if parity else qi) >= 2]
        for (qi, t) in need:
            topk_init(qi, t)
        for itx in range(13):
            for (qi, t) in need:
                topk_iter(qi, t, itx)
        for (qi, t) in pair:
            attend(qi, t)


def norm_rows(k, x_, h_, junk, ss, nfeat):
    k.op("act", lambda e: e.activation(out=junk[:], in_=x_[:], func=AF.Square, accum_out=ss[:]), reads=[x_], writes=[junk, ss])
    k.op("dve", lambda e: e.tensor_scalar(ss[:], ss[:], 1.0 / nfeat, EPS, op0=ALU.mult, op1=ALU.add), reads=[ss], writes=[ss])
    k.op("act", lambda e: e.activation(out=ss[:], in_=ss[:], func=AF.Sqrt), reads=[ss], writes=[ss])
    k.op("dve", lambda e: e.reciprocal(ss[:], ss[:]), reads=[ss], writes=[ss])
    k.op("dve", lambda e: e.tensor_scalar(h_[:], x_[:], ss[:, 0:1], None, op0=ALU.mult), reads=[x_, ss], writes=[h_])


def transpose_rows(k, h_, hT, gbuf, gi, ident, ptr, col0, nkc=16, ncols=128):
    for q in range(nkc // 4):
        p_ = ptr[q % 2]
        for j in range(4):
            kc = q * 4 + j
            k.op("pe", lambda e, p_=p_, kc=kc, j=j: e.transpose(p_[:, j, :], h_[:, kc * 128:(kc + 1) * 128], ident[:]), reads=[h_, ident], writes=[p_])
        for j in range(4):
            kc = q * 4 + j
            dst = hT[:, kc, col0:col0 + ncols]
            src = p_[:, j, 128 - ncols:128]
            if j % 2 == 0:
                k.op("act", lambda e, dst=dst, src=src, kc=kc: e.activation(out=dst, in_=src, func=AF.Copy, scale=gbuf[:, gi, kc:kc + 1]), reads=[p_, gbuf], writes=[hT])
            else:
                k.op("dve", lambda e, dst=dst, src=src, kc=kc: e.tensor_scalar(dst, src, gbuf[:, gi, kc:kc + 1], None, op0=ALU.mult), reads=[p_, gbuf], writes=[hT])


def p3a_body(k, x, sgT, oaT, obT, ocT, w_a, w_b, w_c, w_out, x1):
    P = [k.ps("ps%d" % i, [128, 512], F32) for i in range(8)]
    yT = k.sb("yT", [128, 16, T], BF16)
    st = ExitStack()
    oT = k.sb("oT", [128, 16, T], BF16, stack=st)
    k.dma("sp", oT[:, 0:8, :], oaT.rearrange("(kc p) t -> p kc t", p=128), writes=[oT])
    k.dma("sp", oT[:, 8:12, :], obT.rearrange("(kc p) t -> p kc t", p=128), writes=[oT])
    k.dma("sp", oT[:, 12:16, :], ocT.rearrange("(kc p) t -> p kc t", p=128), writes=[oT])
    wi = [k.sb("wi%d" % i, [128, 16, 256], BF16, stack=st) for i in range(2)]
    sgb = [k.sb("sgb%d" % i, [128, 3, 512], F32, stack=st) for i in range(4)]
    t0 = [k.sb("t0_%d" % i, [128, 512], F32, stack=st) for i in range(2)]
    t1 = [k.sb("t1_%d" % i, [128, 512], F32, stack=st) for i in range(2)]
    it = 0
    for cg in range(8):
        wb = wi[cg % 2]
        wload(k, wb, [(w_a, 0, 8), (w_b, 0, 4), (w_c, 0, 4)], cg * 256, 256)
        for cc in range(2):
            c = cg * 2 + cc
            for tb in range(4):
                sg_ = sgb[it % 4]
                t0_, t1_ = t0[it % 2], t1[it % 2]
                pY = [P[(it % 2) * 3 + i] for i in range(3)]
                it += 1
                k.dma("sp", sg_[:], sgT[:, tb * 512:(tb + 1) * 512].rearrange("(i f) t -> f i t", i=3)[c * 128:(c + 1) * 128], writes=[sg_])
                for i, (k0, k1) in enumerate(((0, 8), (8, 12), (12, 16))):
                    for kc in range(k0, k1):
                        k.op("pe", lambda e, i=i, kc=kc, k0=k0, k1=k1, pY=pY, wb=wb, cc=cc, tb=tb: e.matmul(
                            pY[i][:, :], lhsT=wb[:, kc, cc * 128:(cc + 1) * 128], rhs=oT[:, kc, tb * 512:(tb + 1) * 512],
                            start=(kc == k0), stop=(kc == k1 - 1)), reads=[wb, oT], writes=[pY[i]])
                k.op("dve", lambda e, t0_=t0_, pY=pY, sg_=sg_: e.tensor_tensor(out=t0_[:], in0=pY[0][:], in1=sg_[:, 0, :], op=ALU.mult), reads=[pY[0], sg_], writes=[t0_])
                k.op("dve", lambda e, t1_=t1_, pY=pY, sg_=sg_: e.tensor_tensor(out=t1_[:], in0=pY[1][:], in1=sg_[:, 1, :], op=ALU.mult), reads=[pY[1], sg_], writes=[t1_])
                k.op("dve", lambda e, t0_=t0_, t1_=t1_: e.tensor_tensor(out=t0_[:], in0=t0_[:], in1=t1_[:], op=ALU.add), reads=[t0_, t1_], writes=[t0_])
                k.op("dve", lambda e, t1_=t1_, pY=pY, sg_=sg_: e.tensor_tensor(out=t1_[:], in0=pY[2][:], in1=sg_[:, 2, :], op=ALU.mult), reads=[pY[2], sg_], writes=[t1_])
                k.op("dve", lambda e, t0_=t0_, t1_=t1_, c=c, tb=tb: e.tensor_tensor(out=yT[:, c, tb * 512:(tb + 1) * 512], in0=t0_[:], in1=t1_[:], op=ALU.add),
                     reads=[t0_, t1_], writes=[yT])
    k.barrier()
    st.close()
    wo = k.sb("wo", [128, 16, D], BF16)
    for cg in range(4):
        k.dma("pool", wo[:, :, cg * 512:(cg + 1) * 512], w_out[:, cg * 512:(cg + 1) * 512].rearrange("(kc p) c -> p kc c", p=128), writes=[wo])
    xt = [k.sb("xt%d" % i, [128, D], F32) for i in range(2)]
    for t in range(T // 128):
        x_ = xt[t % 2]
        k.dma("sp", x_[:], x[t * 128:(t + 1) * 128, :], writes=[x_])
        for cb in range(4):
            p_ = P[(t * 4 + cb) % 8]
            for kc in range(16):
                k.op("pe", lambda e, p_=p_, kc=kc, t=t, cb=cb: e.matmul(p_[:, :], lhsT=yT[:, kc, t * 128:(t + 1) * 128], rhs=wo[:, kc, cb * 512:(cb + 1) * 512],
                                                                       start=(kc == 0), stop=(kc == 15)), reads=[yT, wo], writes=[p_])
            k.op("dve", lambda e, p_=p_, x_=x_, cb=cb: e.tensor_tensor(out=x_[:, cb * 512:(cb + 1) * 512], in0=x_[:, cb * 512:(cb + 1) * 512], in1=p_[:, :], op=ALU.add),
                 reads=[x_, p_], writes=[x_])
        k.dma("sp", x1[t * 128:(t + 1) * 128, :], x_[:], reads=[x_])


def p3b_body(k, c, x1, pT_d, gn, gbf_d, w_gate, w_up, w_down, cw_d, cb_d, wpg, wpp, x3, final):
    P = [k.ps("ps%d" % i, [128, 512], F32) for i in range(6)]
    ptr = [k.ps("ptr%d" % i, [128, 4, 128], BF16) for i in range(2)]
    ident = c["ident"]
    gnb = k.sb("gn_s", [128, 3, 16], F32)
    k.dma("sp", gnb[:], gn[:, :, :], writes=[gnb])
    cw = k.sb("cw_s", [128, NFC, 3], F32)
    cb = k.sb("cb_s", [128, NFC], F32)
    k.dma("sp", cw[:], cw_d[:, :, :], writes=[cw])
    k.dma("sp", cb[:], cb_d[:, :], writes=[cb])
    if final:
        gbf = k.sb("gbf_s", [128, D], F32)
        k.dma("sp", gbf[:], gbf_d[:, :], writes=[gbf])
    hTs = [k.sb("hT%d" % i, [128, 16, 514], BF16) for i in range(2)]

    mT = k.sb("mT", [128, NFC, 512], BF16)
    wg = [k.sb("wg%d" % i, [128, 16, 128], BF16) for i in range(2)]
    wu = [k.sb("wu%d" % i, [128, 16, 128], BF16) for i in range(2)]
    wd = [k.sb("wd%d" % i, [128, 22, 512], BF16) for i in range(2)]
    x2 = k.sb("x2", [128, 4, D], F32)
    xt = k.sb("xt", [128, D], F32)
    hb = k.sb("hb", [128, D], BF16)
    junk = hb
    ss = k.sb("ss", [128, 1], F32)
    A_sb = [k.sb("A_sb%d" % i, [128, 514], F32) for i in range(2)]
    cv = [k.sb("cv%d" % i, [128, 512], F32) for i in range(2)]
    gg = [k.sb("gg%d" % i, [128, 512], F32) for i in range(2)]
    sgG = gg
    pTb = k.sb("pTb", [128, 2, 512], BF16)
    def prep(tb):
        hT = hTs[tb % 2]
        if tb == 0:
            k.op("dve", lambda e: e.memset(hT[:, :, 0:2], 0.0), writes=[hT])
        else:
            k.dma("sp", xt[:], x1[tb * 512 - 128: tb * 512, :], writes=[xt])
            norm_rows(k, xt, hb, junk, ss, D)
            transpose_rows(k, hb, hT, gnb, 0, ident, ptr, 0, ncols=2)
        for j in range(4):
            k.dma("sp", xt[:], x1[tb * 512 + j * 128: tb * 512 + (j + 1) * 128, :], writes=[xt])
            norm_rows(k, xt, hb, junk, ss, D)
            transpose_rows(k, hb, hT, gnb, 0, ident, ptr, 2 + j * 128)

    it = 0

    def block(tb):
        nonlocal it
        if tb == 0:
            prep(0)
        hT = hTs[tb % 2]
        for j in range(4):
            k.dma("sp", x2[:, j, :], x1[tb * 512 + j * 128: tb * 512 + (j + 1) * 128, :], writes=[x2])
        for fc in range(NFC):
            if fc == NFC // 2 and tb + 1 < S // 512:
                prep(tb + 1)
            wg_, wu_ = wg[fc % 2], wu[fc % 2]
            wload(k, wg_, [(w_gate, 0, 16)], fc * 128, 128)
            wload(k, wu_, [(w_up, 0, 16)], fc * 128, 128)
            pa, pu, ph = P[(it % 2) * 3], P[(it % 2) * 3 + 1], P[(it % 2) * 3 + 2]
            A_, cv_, gg_ = A_sb[it % 2], cv[it % 2], gg[it % 2]
            it += 1
            for kc in range(16):
                k.op("pe", lambda e, pa=pa, kc=kc, wg_=wg_: e.matmul(pa[:, :], lhsT=wg_[:, kc, :], rhs=hT[:, kc, 2:514], start=(kc == 0), stop=(kc == 15)),
                     reads=[wg_, hT], writes=[pa])
            for kc in range(16):
                k.op("pe", lambda e, ph=ph, kc=kc, wg_=wg_: e.matmul(ph[:, 0:2], lhsT=wg_[:, kc, :], rhs=hT[:, kc, 0:2], start=(kc == 0), stop=(kc == 15)),
                     reads=[wg_, hT], writes=[ph])
            for kc in range(16):
                k.op("pe", lambda e, pu=pu, kc=kc, wu_=wu_: e.matmul(pu[:, :], lhsT=wu_[:, kc, :], rhs=hT[:, kc, 2:514], start=(kc == 0), stop=(kc == 15)),
                     reads=[wu_, hT], writes=[pu])
            k.op("act", lambda e, A_=A_, pa=pa: e.copy(A_[:, 2:514], pa[:, :]), reads=[pa], writes=[A_])
            k.op("act", lambda e, A_=A_, ph=ph: e.copy(A_[:, 0:2], ph[:, 0:2]), reads=[ph], writes=[A_])
            k.op("dve", lambda e, A_=A_, cv_=cv_, fc=fc: e.tensor_scalar(cv_[:], A_[:, 2:514], cw[:, fc, 2:3], cb[:, fc:fc + 1], op0=ALU.mult, op1=ALU.add),
                 reads=[A_, cw, cb], writes=[cv_])
            k.op("dve", lambda e, A_=A_, cv_=cv_, fc=fc: e.scalar_tensor_tensor(out=cv_[:], in0=A_[:, 1:513], scalar=cw[:, fc, 1:2], in1=cv_[:], op0=ALU.mult, op1=ALU.add),
                 reads=[A_, cw, cv_], writes=[cv_])
            k.op("dve", lambda e, A_=A_, cv_=cv_, fc=fc: e.scalar_tensor_tensor(out=cv_[:], in0=A_[:, 0:512], scalar=cw[:, fc, 0:1], in1=cv_[:], op0=ALU.mult, op1=ALU.add),
                 reads=[A_, cw, cv_], writes=[cv_])
            k.op("act", lambda e, cv_=cv_, gg_=gg_: e.activation(out=gg_[:], in_=cv_[:], func=AF.Gelu_apprx_tanh), reads=[cv_], writes=[gg_])
            k.op("dve", lambda e, gg_=gg_, pu=pu, fc=fc: e.tensor_tensor(out=mT[:, fc, :], in0=gg_[:], in1=pu[:, :], op=ALU.mult), reads=[gg_, pu], writes=[mT])
        for cbk in range(4):
            pd = [P[j] for j in range(4)]
            for piece in range(2):
                wd_ = wd[(cbk * 2 + piece) % 2]
                wload(k, wd_, [(w_down, piece * 22 * 128, 22)], cbk * 512, 512)
                for j in range(4):
                    for f in range(22):
                        fc = piece * 22 + f
                        k.op("pe", lambda e, j=j, f=f, fc=fc, wd_=wd_, pd=pd: e.matmul(pd[j][:, :], lhsT=mT[:, fc, j * 128:(j + 1) * 128], rhs=wd_[:, f, :],
                                                                                  start=(fc == 0), stop=(fc == NFC - 1)), reads=[mT, wd_], writes=[pd[j]])
            for j in range(4):
                k.op("dve", lambda e, j=j, cbk=cbk, pd=pd: e.tensor_tensor(out=x2[:, j, cbk * 512:(cbk + 1) * 512], in0=x2[:, j, cbk * 512:(cbk + 1) * 512], in1=pd[j][:, :], op=ALU.add),
                     reads=[x2, pd[j]], writes=[x2])
        for j in range(4):
            k.op("act", lambda e, j=j: e.copy(xt[:], x2[:, j, :]), reads=[x2], writes=[xt])
            norm_rows(k, xt, hb, junk, ss, D)
            transpose_rows(k, hb, hT, gnb, 1, ident, ptr, 2 + j * 128)
        k.dma("pool", pTb[:], pT_d[:, tb * 512:(tb + 1) * 512].rearrange("(kc p) t -> p kc t", p=128), writes=[pTb])
        for cbk in range(4):
            wd_ = wd[cbk % 2]
            wload(k, wd_, [(wpg, 0, 16), (wpp, 0, 2)], cbk * 512, 512)
            for j in range(4):
                pG, pP = P[4 + (j % 2)], P[j % 2]
                sg_ = sgG[j % 2]
                for kc in range(16):
                    k.op("pe", lambda e, pG=pG, kc=kc, j=j, wd_=wd_: e.matmul(pG[:, :], lhsT=hT[:, kc, 2 + j * 128: 2 + (j + 1) * 128], rhs=wd_[:, kc, :],
                                                                           start=(kc == 0), stop=(kc == 15)), reads=[hT, wd_], writes=[pG])
                for kc in range(2):
                    k.op("pe", lambda e, pP=pP, kc=kc, j=j, wd_=wd_: e.matmul(pP[:, :], lhsT=pTb[:, kc, j * 128:(j + 1) * 128], rhs=wd_[:, 16 + kc, :],
                                                                           start=(kc == 0), stop=(kc == 1)), reads=[pTb, wd_], writes=[pP])
                k.op("act", lambda e, pG=pG, sg_=sg_: e.activation(out=sg_[:], in_=pG[:, :], func=AF.Sigmoid), reads=[pG], writes=[sg_])
                k.op("dve", lambda e, pP=pP, sg_=sg_: e.tensor_tensor(out=sg_[:], in0=sg_[:], in1=pP[:, :], op=ALU.mult), reads=[pP, sg_], writes=[sg_])
                k.op("dve", lambda e, sg_=sg_, j=j, cbk=cbk: e.tensor_tensor(out=x2[:, j, cbk * 512:(cbk + 1) * 512], in0=x2[:, j, cbk * 512:(cbk + 1) * 512], in1=sg_[:], op=ALU.add),
                     reads=[x2, sg_], writes=[x2])
        for j in range(4):
            if final:
                k.op("act", lambda e, j=j: e.activation(out=junk[:], in_=x2[:, j, :], func=AF.Square, accum_out=ss[:]), reads=[x2], writes=[junk, ss])
                k.op("dve", lambda e: e.tensor_scalar(ss[:], ss[:], 1.0 / D, EPS, op0=ALU.mult, op1=ALU.add), reads=[ss], writes=[ss])
                k.op("act", lambda e: e.activation(out=ss[:], in_=ss[:], func=AF.Sqrt), reads=[ss], writes=[ss])
                k.op("dve", lambda e: e.reciprocal(ss[:], ss[:]), reads=[ss], writes=[ss])
                k.op("dve", lambda e, j=j: e.scalar_tensor_tensor(out=x2[:, j, :], in0=x2[:, j, :], scalar=ss[:, 0:1], in1=gbf[:], op0=ALU.mult, op1=ALU.mult),
                     reads=[x2, ss, gbf], writes=[x2])
            k.dma("sp", x3[tb * 512 + j * 128: tb * 512 + (j + 1) * 128, :], x2[:, j, :], reads=[x2])

    for tb_ in range(S // 512):
        block(tb_)


W_SHAPES = [
    ("w_in", [D, INC]), ("gb", [128, D]), ("kvg", [128, 256]), ("wuk", [256, 128]), ("wuv", [256, 128]),
    ("ck_w1", [4096, 256]), ("ck_w2", [256, 128]), ("ck_peT", [128, 32]),
    ("cv_w1", [4096, 256]), ("cv_w2", [256, 128]), ("cv_peT", [128, 32]),
    ("w_a", [1024, D]), ("w_b", [512, D]), ("w_c", [512, D]), ("w_out", [D, D]),
    ("gn", [128, 3, 16]), ("w_gate", [D, DFF]), ("w_up", [D, DFF]), ("w_down", [DFF, D]),
    ("cw", [128, NFC, 3]), ("cb", [128, NFC]), ("wpg", [D, D]), ("wpp", [256, D]),
]
C_SHAPES = [
    ("wslc", [2, 128, 4, 1024], F32), ("wwin", [2, 128, 4, 1408], F32), ("mext", [2, 128, 4, 503], F32), ("nc31", [2, 128, 4], F32),
    ("keep", [128, 128], F32), ("add", [128, 128], F32), ("ov", [128, 2, 64], BF16), ("eexp", [64, S], BF16),
    ("dbias", [128, 4, 2, 128], F32), ("dc31", [128, 4], F32), ("dcaus", [128, 128], F32), ("gbf", [128, D], F32),
]


def build_prog(nl, final):
    k = KB()
    c = consts_common(k)
    x_in = k.dram_in("x", [S, D], F32)
    pT = k.dram_in("pT", [nl, 256, S], F32)
    W = {nm: k.dram_in(nm, [nl] + shp, F32) for nm, shp in W_SHAPES}
    C = {nm: k.dram_in(nm, shp, dt) for nm, shp, dt in C_SHAPES}
    out = k.dram_out("out", [S, D], F32)
    sc = {}
    for (name, c0, n, mode, dt, act) in SEGS:
        if mode == "FM":
            sc[name] = k.dram_tmp("t_" + name, [n, S], dt)
        elif mode == "TM":
            sc[name] = k.dram_tmp("t_" + name, [S, n], dt)
    sc["dkT"] = k.dram_tmp("t_dkT", [128, S], BF16)
    sc["dv"] = k.dram_tmp("t_dv", [S, 128], BF16)
    sc["oaT"] = k.dram_tmp("t_oaT", [1024, S], BF16)
    sc["obT"] = k.dram_tmp("t_obT", [512, S], BF16)
    sc["ocT"] = k.dram_tmp("t_ocT", [512, S], BF16)
    X1 = k.dram_tmp("t_X1", [S, D], F32)
    XS = [k.dram_tmp("t_XS%d" % i, [S, D], F32) for i in range(2)]
    for li in range(nl):
        xin = x_in if li == 0 else XS[(li - 1) % 2]
        xout = out if li == nl - 1 else XS[li % 2]
        for half in range(2):
            k.begin()
            p1_body(k, c, xin, W["w_in"][li], W["gb"][li], W["kvg"][li], W["wuk"][li], W["wuv"][li], sc, half * T)
            k.end()
        for g in range(2):
            k.begin()
            d = {"qnT": sc["qnT"][g * 512:(g + 1) * 512, :], "ng": sc["ng"][:, g * 12:(g + 1) * 12]}
            for nm in ("kcT", "vcT", "ksT", "kwT"):
                d[nm] = sc[nm][g * 128:(g + 1) * 128, :]
            for nm in ("vs", "vw"):
                d[nm] = sc[nm][:, g * 128:(g + 1) * 128]
            for nm in ("ck_w1", "ck_w2", "ck_peT", "cv_w1", "cv_w2", "cv_peT"):
                d[nm] = W[nm][li]
            for nm in ("wslc", "wwin", "mext", "nc31"):
                d[nm] = C[nm][g]
            for nm in ("keep", "add", "ov", "eexp"):
                d[nm] = C[nm]
            build_nsa(k, c, d, sc["oaT"][g * 512:(g + 1) * 512, :])
            k.end()
        k.begin()
        build_sb(k, c, sc["sbqT"], sc["sbkT"], sc["sbv"], sc["obT"], 4)
        k.end()
        k.begin()
        build_dsa(k, c, sc["dqT"], sc["dkT"], sc["dv"], sc["iqT"], sc["ikT"], sc["iw"], C["dbias"], C["dc31"], C["dcaus"], sc["ocT"], nqt=32, near=2, parity=False)
        k.end()
        for half in range(2):
            ts = slice(half * T, (half + 1) * T)
            k.begin()
            p3a_body(k, xin[ts, :], sc["sgT"][:, ts], sc["oaT"][:, ts], sc["obT"][:, ts], sc["ocT"][:, ts],
                     W["w_a"][li], W["w_b"][li], W["w_c"][li], W["w_out"][li], X1[ts, :])
            k.end()
        k.begin()
        p3b_body(k, c, X1, pT[li], W["gn"][li], C["gbf"], W["w_gate"][li], W["w_up"][li], W["w_down"][li], W["cw"][li], W["cb"][li],
                 W["wpg"][li], W["wpp"][li], xout, final and li == nl - 1)
        k.end()
    return k.finish()


def _bc(v, rows=128):
    return np.ascontiguousarray(np.broadcast_to(np.asarray(v, np.float32)[None, :], (rows, v.shape[0])))


def _pp(v):
    return np.ascontiguousarray(np.asarray(v, np.float32).reshape(-1, 128).T)


def host_layout(inp, layers):
    Ls = list(layers)
    g = lambda nm: np.asarray(inp[nm], np.float32)
    Wd = {
        "w_in": g("w_in")[Ls], "wuk": g("dsa_w_uk")[Ls], "wuv": g("dsa_w_uv")[Ls],
        "ck_w1": g("cmp_k_w1")[Ls], "ck_w2": g("cmp_k_w2")[Ls], "cv_w1": g("cmp_v_w1")[Ls], "cv_w2": g("cmp_v_w2")[Ls],
        "w_a": g("w_proj_a")[Ls], "w_b": g("w_proj_b")[Ls], "w_c": g("w_proj_c")[Ls], "w_out": g("w_out")[Ls],
        "w_gate": g("ffn_w_gate")[Ls], "w_up": g("ffn_w_up")[Ls], "w_down": g("ffn_w_down")[Ls], "wpg": g("ple_w_gate")[Ls], "wpp": g("ple_w_proj")[Ls],
    }
    Wd["gb"] = np.stack([_bc(g("norm_mix")[L]) for L in Ls])
    Wd["kvg"] = np.stack([_bc(g("dsa_kv_norm")[L]) for L in Ls])
    Wd["ck_peT"] = np.stack([np.ascontiguousarray(g("cmp_k_pe")[L].T) for L in Ls])
    Wd["cv_peT"] = np.stack([np.ascontiguousarray(g("cmp_v_pe")[L].T) for L in Ls])
    Wd["gn"] = np.stack([np.stack([_pp(g("norm_ffn")[L]), _pp(g("norm_ple")[L]), _pp(g("norm_final"))], 1) for L in Ls])
    Wd["cw"] = np.stack([np.ascontiguousarray(g("ffn_conv_w")[L].reshape(3, NFC, 128).transpose(2, 1, 0)) for L in Ls])
    Wd["cb"] = np.stack([np.ascontiguousarray(g("ffn_conv_b")[L].reshape(NFC, 128).T) for L in Ls])
    tab = g("rel_bias_table")
    n0, n1 = nsa_consts(tab, 0), nsa_consts(tab, 1)
    Cd = {nm: np.stack([n0[nm], n1[nm]]) for nm in ("wslc", "wwin", "mext", "nc31")}
    Cd["keep"], Cd["add"] = n0["keep"], n0["add"]
    Cd["ov"], Cd["eexp"] = n0["ov"].astype(NPBF), n0["eexp"].astype(NPBF)
    Cd["dbias"], Cd["dc31"], Cd["dcaus"] = dsa_consts_exact(tab)
    Cd["gbf"] = _bc(g("norm_final"))
    return Wd, Cd


_PROGS = {}


def _prog(nl, final):
    key = (nl, final)
    if key not in _PROGS:
        _PROGS[key] = build_prog(nl, final)
    return _PROGS[key]


LAYERS_PER_LAUNCH = 4


def kernel(**inp):
    x = np.asarray(inp["x"], np.float32)
    p = np.asarray(inp["p"], np.float32)
    B = x.shape[0]
    depth = p.shape[0]
    cur = [np.ascontiguousarray(x[b]) for b in range(B)]
    for l0 in range(0, depth, LAYERS_PER_LAUNCH):
        Ls = list(range(l0, min(depth, l0 + LAYERS_PER_LAUNCH)))
        final = (Ls[-1] == depth - 1)
        Wd, Cd = host_layout(inp, Ls)
        nc = _prog(len(Ls), final)
        in_maps = []
        for b in range(B):
            m = {"x": cur[b], "pT": np.ascontiguousarray(np.stack([p[L, b].T for L in Ls]))}
            m.update(Wd)
            m.update(Cd)
            in_maps.append(m)
        res = run_bass_kernel_spmd(nc, in_maps, core_ids=list(range(B)))
        cur = [np.asarray(res.results[b]["out"], np.float32) for b in range(B)]
    return np.stack(cur).astype(np.float32)
```
